# Optimizing a Trainium2 kernel written in Bass

```python
import math
import jax, jax.numpy as jnp
from jax import lax
import numpy as np

D_MODEL = 4096
BATCH = 8
SEQ = 2048
DEPTH = 4

GLA_HEADS = 8
GLA_DK = 128
GLA_DV = 192
GLA_RANK = 16
GLA_TAU = 16.0
GLA_CHUNK = 64
SG_GROUPS = 8
SG_GROUP_WIDTH = 128
SG_CHUNK = 128
DIFF_HEADS = 6
DIFF_DK = 128
DIFF_DV = 256
ATTN_BLOCK = 128
D_FF = 6144
CONV_WIDTH = 3
N_BRANCH = 3
EPS = 1e-6

GLA_QK = GLA_HEADS * GLA_DK
GLA_V = GLA_HEADS * GLA_DV
SG_W = SG_GROUPS * SG_GROUP_WIDTH
DIFF_QK = DIFF_HEADS * 2 * DIFF_DK
DIFF_V = DIFF_HEADS * DIFF_DV
SPLIT_SIZES = (GLA_QK, GLA_QK, GLA_V, GLA_V, GLA_RANK, SG_W, SG_W, DIFF_QK, DIFF_QK, DIFF_V, N_BRANCH * D_MODEL)
IN_COLS = sum(SPLIT_SIZES)

kernel_name = "hybrid_gla_gmlp_diffattn_convffn"


def rmsnorm(x, g):
    xf = x.astype(jnp.float32)
    y = xf * lax.rsqrt(jnp.mean(xf * xf, axis=-1, keepdims=True) + EPS)
    return (y * g.astype(jnp.float32)).astype(x.dtype)


def layernorm(x, g, b):
    xf = x.astype(jnp.float32)
    mu = jnp.mean(xf, axis=-1, keepdims=True)
    xc = xf - mu
    var = jnp.mean(xc * xc, axis=-1, keepdims=True)
    return (xc * lax.rsqrt(var + EPS) * g.astype(jnp.float32) + b.astype(jnp.float32)).astype(x.dtype)


def alibi_slopes(n):
    def pow2(m):
        start = 2.0 ** (-8.0 / m)
        return [start ** (i + 1) for i in range(m)]
    if math.log2(n).is_integer():
        s = pow2(n)
    else:
        p = 2 ** int(math.floor(math.log2(n)))
        s = pow2(p) + pow2(2 * p)[0::2][: n - p]
    return jnp.asarray(np.array(s, dtype=np.float32))


def gla_chunked(q, k, v, log_a):
    B, S, H, DK = q.shape
    DV = v.shape[-1]
    C = GLA_CHUNK
    N = S // C

    def to_chunks(t):
        return t.reshape(B, N, C, H, t.shape[-1]).transpose(1, 0, 3, 2, 4).astype(jnp.float32)

    qc = to_chunks(q) * (DK ** -0.5)
    kc = to_chunks(k)
    vc = to_chunks(v)
    bc = jnp.cumsum(to_chunks(log_a), axis=3)
    causal = jnp.tril(jnp.ones((C, C), dtype=bool))[:, :, None]

    def step(state, inp):
        q_, k_, v_, b_ = inp
        diff = b_[:, :, :, None, :] - b_[:, :, None, :, :]
        decay = jnp.exp(jnp.where(causal, diff, -jnp.inf))
        attn = jnp.einsum('bhtd,bhsd,bhtsd->bhts', q_, k_, decay)
        o = jnp.einsum('bhts,bhsv->bhtv', attn, v_) + jnp.einsum('bhtd,bhdv->bhtv', q_ * jnp.exp(b_), state)
        b_last = b_[:, :, -1:, :]
        k_dec = k_ * jnp.exp(b_last - b_)
        state = state * jnp.exp(b_last[:, :, 0, :])[..., None] + jnp.einsum('bhsd,bhsv->bhdv', k_dec, v_)
        return state, o

    s0 = jnp.zeros((B, H, DK, DV), jnp.float32)
    _, o = lax.scan(step, s0, (qc, kc, vc, bc))
    return o.transpose(1, 0, 3, 2, 4).reshape(B, S, H, DV)


def spatial_gating(u, v, ln_g, ln_b, w_s, b_s):
    B, S, _ = u.shape
    N = S // SG_CHUNK
    u = jax.nn.gelu(u)
    v = layernorm(jax.nn.gelu(v), ln_g, ln_b)
    vc = v.reshape(B, N, SG_CHUNK, SG_GROUPS, SG_GROUP_WIDTH)
    w = w_s * jnp.tril(jnp.ones((SG_CHUNK, SG_CHUNK), dtype=w_s.dtype))
    f = jnp.einsum('gts,bnsgc->bntgc', w, vc) + b_s.T[None, None, :, :, None]
    return u * f.reshape(B, S, SG_W)


def diff_attention(q, k, v, lam, slopes):
    B, S, H, _, DK = q.shape
    DV = v.shape[-1]
    NB = S // ATTN_BLOCK
    qb = q.reshape(B, NB, ATTN_BLOCK, H, 2, DK).transpose(1, 0, 2, 3, 4, 5)
    kf = k.astype(jnp.float32)
    vf = v.astype(jnp.float32)
    key_pos = jnp.arange(S)
    scale = DK ** -0.5

    def block(args):
        q_blk, i = args
        q_pos = i * ATTN_BLOCK + jnp.arange(ATTN_BLOCK)
        dist = (q_pos[:, None] - key_pos[None, :]).astype(jnp.float32)
        bias = jnp.where(dist[None] >= 0, -slopes[:, None, None] * dist[None], -jnp.inf)
        s = jnp.einsum('bqhmd,bkhmd->bhmqk', q_blk.astype(jnp.float32), kf) * scale + bias[None, :, None]
        p = jax.nn.softmax(s, axis=-1)
        a = p[:, :, 0] - lam * p[:, :, 1]
        return jnp.einsum('bhqk,bkhv->bqhv', a, vf)

    o = lax.map(block, (qb, jnp.arange(NB)))
    return o.transpose(1, 0, 2, 3, 4).reshape(B, S, H, DV)


def causal_dwconv(x, w, b):
    S = x.shape[1]
    xp = jnp.pad(x, ((0, 0), (CONV_WIDTH - 1, 0), (0, 0)))
    y = b
    for j in range(CONV_WIDTH):
        y = y + w[j] * xp[:, j:j + S, :]
    return y


def setup_inputs(seed: int = 0) -> dict:
    key = jax.random.key(seed)
    ks = jax.random.split(key, 32)
    L, D = DEPTH, D_MODEL
    f32 = jnp.float32

    def nrm(k, shape, scale):
        return jax.random.normal(k, shape, f32) * scale

    def gain(k, shape):
        return 1.0 + 0.05 * jax.random.normal(k, shape, f32)

    return {
        "x": jax.random.normal(ks[0], (BATCH, SEQ, D), f32),
        "g_pre_mix": gain(ks[1], (L, D)),
        "w_in": nrm(ks[2], (L, D, IN_COLS), D ** -0.5),
        "w_gla_lr": nrm(ks[3], (L, GLA_RANK, GLA_QK), GLA_RANK ** -0.5),
        "b_gla_lr": nrm(ks[4], (L, GLA_QK), 0.1),
        "gla_norm_g": gain(ks[5], (L, GLA_DV)),
        "sg_ln_g": gain(ks[6], (L, SG_W)),
        "sg_ln_b": nrm(ks[7], (L, SG_W), 0.02),
        "sg_w_s": nrm(ks[8], (L, SG_GROUPS, SG_CHUNK, SG_CHUNK), SG_CHUNK ** -0.5),
        "sg_b_s": gain(ks[9], (L, SG_GROUPS, SG_CHUNK)),
        "diff_lambda_q1": nrm(ks[10], (L, DIFF_DK), 0.1),
        "diff_lambda_k1": nrm(ks[11], (L, DIFF_DK), 0.1),
        "diff_lambda_q2": nrm(ks[12], (L, DIFF_DK), 0.1),
        "diff_lambda_k2": nrm(ks[13], (L, DIFF_DK), 0.1),
        "diff_norm_g": gain(ks[14], (L, DIFF_DV)),
        "w_br_gla": nrm(ks[15], (L, GLA_V, D), GLA_V ** -0.5),
        "w_br_sg": nrm(ks[16], (L, SG_W, D), SG_W ** -0.5),
        "w_br_diff": nrm(ks[17], (L, DIFF_V, D), DIFF_V ** -0.5),
        "w_o": nrm(ks[18], (L, D, D), D ** -0.5),
        "g_post_mix": gain(ks[19], (L, D)),
        "g_pre_ffn": gain(ks[20], (L, D)),
        "w_up": nrm(ks[21], (L, D, 2 * D_FF), D ** -0.5),
        "conv_w": nrm(ks[22], (L, CONV_WIDTH, 2 * D_FF), CONV_WIDTH ** -0.5),
        "conv_b": nrm(ks[23], (L, 2 * D_FF), 0.02),
        "w_down": nrm(ks[24], (L, D_FF, D), D_FF ** -0.5),
        "g_post_ffn": gain(ks[25], (L, D)),
    }


def reference(x, g_pre_mix, w_in, w_gla_lr, b_gla_lr, gla_norm_g, sg_ln_g, sg_ln_b, sg_w_s, sg_b_s,
              diff_lambda_q1, diff_lambda_k1, diff_lambda_q2, diff_lambda_k2, diff_norm_g,
              w_br_gla, w_br_sg, w_br_diff, w_o, g_post_mix, g_pre_ffn, w_up, conv_w, conv_b, w_down, g_post_ffn):
    B, S, D = x.shape
    offsets = np.cumsum(np.array(SPLIT_SIZES))[:-1].tolist()
    slopes = alibi_slopes(DIFF_HEADS)
    for l in range(DEPTH):
        h = rmsnorm(x, g_pre_mix[l])
        proj = h @ w_in[l]
        (gq, gk, gv, gg, glr, su, sv, dq, dk, dv, gates) = jnp.split(proj, offsets, axis=-1)

        gate_logit = (glr @ w_gla_lr[l] + b_gla_lr[l]).astype(jnp.float32)
        log_a = jax.nn.log_sigmoid(gate_logit) / GLA_TAU
        o_gla = gla_chunked(gq.reshape(B, S, GLA_HEADS, GLA_DK), gk.reshape(B, S, GLA_HEADS, GLA_DK),
                            gv.reshape(B, S, GLA_HEADS, GLA_DV), log_a.reshape(B, S, GLA_HEADS, GLA_DK))
        o_gla = rmsnorm(o_gla, gla_norm_g[l]).astype(x.dtype) * jax.nn.silu(gg.reshape(B, S, GLA_HEADS, GLA_DV))
        o_gla = o_gla.reshape(B, S, GLA_V)

        o_sg = spatial_gating(su, sv, sg_ln_g[l], sg_ln_b[l], sg_w_s[l], sg_b_s[l])

        lam_init = 0.8 - 0.6 * math.exp(-0.3 * l)
        lam = (jnp.exp(jnp.sum(diff_lambda_q1[l].astype(jnp.float32) * diff_lambda_k1[l].astype(jnp.float32)))
               - jnp.exp(jnp.sum(diff_lambda_q2[l].astype(jnp.float32) * diff_lambda_k2[l].astype(jnp.float32)))
               + lam_init)
        o_diff = diff_attention(dq.reshape(B, S, DIFF_HEADS, 2, DIFF_DK), dk.reshape(B, S, DIFF_HEADS, 2, DIFF_DK),
                                dv.reshape(B, S, DIFF_HEADS, DIFF_DV), lam, slopes)
        o_diff = (rmsnorm(o_diff, diff_norm_g[l]) * (1.0 - lam_init)).astype(x.dtype).reshape(B, S, DIFF_V)

        gate = jax.nn.sigmoid(gates.reshape(B, S, N_BRANCH, D))
        merged = (gate[:, :, 0] * (o_gla @ w_br_gla[l])
                  + gate[:, :, 1] * (o_sg @ w_br_sg[l])
                  + gate[:, :, 2] * (o_diff @ w_br_diff[l]))
        x = x + rmsnorm(merged @ w_o[l], g_post_mix[l])

        h = rmsnorm(x, g_pre_ffn[l])
        up = causal_dwconv(h @ w_up[l], conv_w[l], conv_b[l])
        a, u = jnp.split(up, 2, axis=-1)
        f = (jax.nn.gelu(a, approximate=True) * u) @ w_down[l]
        x = x + rmsnorm(f, g_post_ffn[l])
    return x
```

```python
import math
import contextlib
import numpy as np
import concourse.bass as bass
import concourse.mybir as mybir
from concourse.bass_utils import run_bass_kernel_spmd

F32 = mybir.dt.float32
BF16 = mybir.dt.bfloat16
AF = mybir.ActivationFunctionType
ALU = mybir.AluOpType
AX = mybir.AxisListType

D = 4096
GLA_H, GLA_DK, GLA_DV, GLA_R = 8, 128, 192, 16
SGW = 1024
DIFF_H, DIFF_DK, DIFF_DV = 6, 128, 256
DFF = 6144
EPS = 1e-6
GLA_QK = GLA_H * GLA_DK
GLA_V = GLA_H * GLA_DV
DIFF_QK = DIFF_H * 2 * DIFF_DK
DIFF_V = DIFF_H * DIFF_DV
SPLIT = (GLA_QK, GLA_QK, GLA_V, GLA_V, GLA_R, SGW, SGW, DIFF_QK, DIFF_QK, DIFF_V, 3 * D)
OFFS = [0]
for _s in SPLIT:
    OFFS.append(OFFS[-1] + _s)
IN_COLS = OFFS[-1]
O_GQ, O_GK, O_GV, O_GG, O_LR, O_SU, O_SV, O_DQ, O_DK, O_DV, O_GT = OFFS[:11]


def alibi_slopes(n):
    def pow2(m):
        start = 2.0 ** (-8.0 / m)
        return [start ** (i + 1) for i in range(m)]
    if math.log2(n).is_integer():
        return pow2(n)
    p = 2 ** int(math.floor(math.log2(n)))
    return pow2(p) + pow2(2 * p)[0::2][: n - p]


class Tok:
    __slots__ = ("sem", "val")

    def __init__(self, sem, val):
        self.sem = sem
        self.val = val


class Eng:
    def __init__(self, nc, eng, name):
        self.eng = eng
        self.sem = nc.alloc_semaphore("sem_" + name)
        self.cnt = 0
        self.seen = {}

    def wait(self, *toks):
        for t in toks:
            if t is None:
                continue
            if isinstance(t, (list, tuple)):
                self.wait(*t)
                continue
            k = id(t.sem)
            if self.seen.get(k, 0) >= t.val:
                continue
            self.eng.wait_ge(t.sem, t.val)
            self.seen[k] = t.val

    def sig(self, ins):
        ins.then_inc(self.sem, 1)
        self.cnt += 1
        return Tok(self.sem, self.cnt)


class DSem:
    def __init__(self, sem):
        self.sem = sem
        self.cnt = 0

    def tok(self):
        return Tok(self.sem, self.cnt) if self.cnt else None


class Ring:
    def __init__(self, tiles):
        self.tiles = tiles
        self.war = [None] * len(tiles)
        self.k = 0

    def next(self):
        i = self.k % len(self.tiles)
        self.k += 1
        return i, self.tiles[i], self.war[i]

    def release(self, i, tok):
        self.war[i] = tok


class Builder:
    def __init__(self, S, L, NC, TT=512, dbg=()):
        self.S, self.L, self.NC, self.TT = S, L, NC, TT
        self.NTT = S // TT
        self.NCH = S // 128
        self.dbg = dbg
        self.serial_cc = False
        nc = self.nc = bass.Bass("TRN2", target_bir_lowering=False)
        self.PE = Eng(nc, nc.tensor, "pe")
        self.ACT = Eng(nc, nc.scalar, "act")
        self.DVE = Eng(nc, nc.vector, "dve")
        self.POOL = Eng(nc, nc.gpsimd, "pool")
        self.SP = Eng(nc, nc.sync, "sp")
        self.engs = [self.PE, self.ACT, self.DVE, self.POOL, self.SP]
        self.ds_free = []
        self.ds_used = []
        self.es = None
        self.uid = 0

    def name(self, p):
        self.uid += 1
        return f"{p}_{self.uid}"

    def ds(self):
        if self.ds_free:
            d = self.ds_free.pop()
        else:
            d = DSem(self.nc.alloc_semaphore(self.name("ds")))
        self.ds_used.append(d)
        return d

    def sb(self, shape, dtype, name="t"):
        return self.es.enter_context(self.nc.sbuf_tensor(self.name(name), list(shape), dtype))

    def ps(self, shape, dtype, name="p"):
        return self.es.enter_context(self.nc.psum_tensor(self.name(name), list(shape), dtype))

    def dma(self, out, in_, ds, waits=(), q=None):
        q = q or self.SP
        q.wait(*waits)
        q.eng.dma_start(out=out, in_=in_).then_inc(ds.sem, 16)
        ds.cnt += 16
        return Tok(ds.sem, ds.cnt)

    @contextlib.contextmanager
    def phase(self):
        with contextlib.ExitStack() as es:
            self.es = es
            yield
            self.phase_end()
        self.es = None

    def phase_end(self):
        nc = self.nc
        for d in self.ds_used:
            t = d.tok()
            if t is not None:
                (self.POOL if getattr(d, "pool", False) else self.SP).wait(t)
        nc.all_engine_barrier()
        for d in self.ds_used:
            if d.cnt:
                nc.sync.sem_clear(d.sem)
                d.cnt = 0
        for e in self.engs:
            if e.cnt:
                nc.sync.sem_clear(e.sem)
                e.cnt = 0
            e.seen = {}
        nc.all_engine_barrier()
        self.ds_free.extend(self.ds_used)
        self.ds_used = []

    def make_wring(self, nslots, elems):
        tiles = [self.sb([128, elems], BF16, "wr") for _ in range(nslots)]
        r = Ring(tiles)
        r.dsem = [self.ds() for _ in range(nslots)]
        return r

    def make_psring(self, n):
        return Ring([self.ps([128, 512], F32, "psg") for _ in range(n)])

    def run_panels(self, wring, psring, panels, ncols):
        nc, PE, SP = self.nc, self.PE, self.SP
        ns = len(wring.tiles)
        pre = ns - 1

        def issue(i):
            P = panels[i]
            s, tile, war = wring.next()
            SP.wait(war)
            KC, w = P["KC"], P["w"]
            view = tile[:, 0:KC * w].rearrange("p (k n) -> p k n", n=w)
            src = P["W"].ap().rearrange("(k p) n -> p k n", p=128)
            tok = None
            for k0 in range(0, KC, 8):
                k1 = min(KC, k0 + 8)
                tok = self.dma(view[:, k0:k1, :], src[:, k0:k1, P["c0"]:P["c0"] + w], wring.dsem[s])
            P["slot"], P["view"], P["ltok"] = s, view, tok

        for i in range(min(pre, len(panels))):
            issue(i)
        for i, P in enumerate(panels):
            if i + pre < len(panels):
                issue(i + pre)
            PE.wait(P["ltok"])
            KC, w, view, act = P["KC"], P["w"], P["view"], P["act"]
            last = None
            if P["form"] == "F":
                nb = (w + 127) // 128
                for j in range(nb):
                    m = min(128, w - j * 128)
                    pi, pst, pwar = psring.next()
                    PE.wait(pwar)
                    for kc in range(KC):
                        ins = nc.tensor.matmul(pst[0:m, 0:ncols], lhsT=view[:, kc, j * 128:j * 128 + m],
                                               rhs=act[:, kc, 0:ncols], start=(kc == 0), stop=(kc == KC - 1))
                    last = PE.sig(ins)
                    psring.release(pi, P["evac"](j, pst, last))
            else:
                for mi in range(ncols // 128):
                    pi, pst, pwar = psring.next()
                    PE.wait(pwar)
                    for kc in range(KC):
                        ins = nc.tensor.matmul(pst[:, 0:w], lhsT=act[:, kc, mi * 128:(mi + 1) * 128],
                                               rhs=view[:, kc, :], start=(kc == 0), stop=(kc == KC - 1))
                    last = PE.sig(ins)
                    psring.release(pi, P["evac"](mi, pst, last))
            wring.release(P["slot"], last)

    def make_stage(self, n, shape, dtype):
        r = Ring([self.sb(shape, dtype, "stg") for _ in range(n)])
        r.dsem = [self.ds() for _ in range(n)]
        return r

    def evac_store(self, stage, src_ap, dst_ap, view, mm_tok, func=None, eng=None):
        si, st, _ = stage.next()
        war = stage.dsem[si].tok()
        if func is not None:
            eng = self.ACT
        if eng is None:
            eng = self.ACT if (stage.k % 2 == 0) else self.DVE
        eng.wait(mm_tok, war)
        o = view(st)
        if eng is self.ACT:
            ins = self.nc.scalar.activation(out=o, in_=src_ap, func=func or AF.Copy)
        else:
            ins = self.nc.vector.tensor_copy(out=o, in_=src_ap)
        tok = eng.sig(ins)
        self.dma(dst_ap, o, stage.dsem[si], waits=[tok])
        return tok

    def norm_rstd(self, src, t0, ncols, xring, sqring, psn, ones, rstd):
        nc, PE, ACT, DVE = self.nc, self.PE, self.ACT, self.DVE
        KC = D // 128
        ptok = None
        for kc in range(KC):
            xi, xt, xwar = xring.next()
            lt = self.dma(xt[:, 0:ncols], src[kc * 128:(kc + 1) * 128, t0:t0 + ncols], xring.dsem[xi], waits=[xwar])
            qi, qt, qwar = sqring.next()
            ACT.wait(lt, qwar)
            at = ACT.sig(nc.scalar.activation(out=qt[:, 0:ncols], in_=xt[:, 0:ncols], func=AF.Square))
            xring.release(xi, at)
            PE.wait(at)
            if kc == 0:
                PE.wait(self.psn_war)
            ptok = PE.sig(nc.tensor.matmul(psn[:, 0:ncols], lhsT=ones, rhs=qt[:, 0:ncols],
                                           start=(kc == 0), stop=(kc == KC - 1)))
            sqring.release(qi, ptok)
        ACT.wait(ptok, self.rstd_war)
        at = ACT.sig(nc.scalar.activation(out=rstd[:, 0:ncols], in_=psn[:, 0:ncols], func=AF.Sqrt,
                                          scale=1.0 / D, bias=EPS))
        self.psn_war = at
        DVE.wait(at)
        rt = DVE.sig(nc.vector.reciprocal(out=rstd[:, 0:ncols], in_=rstd[:, 0:ncols]))
        return rt

    def make_xring(self, n, ncols):
        r = Ring([self.sb([128, ncols], F32, "xr") for _ in range(n)])
        r.dsem = [self.ds() for _ in range(n)]
        return r

    def declare(self):
        nc, S, L, NC = self.nc, self.S, self.L, self.NC
        self.x_in = nc.dram_tensor("x", [S, D], F32, kind="ExternalInput")
        self.y_out = nc.dram_tensor("y", [S, D], F32, kind="ExternalOutput")
        self.wspec = {"w_in": (D, IN_COLS), "w_br_gla": (GLA_V, D), "w_br_sg": (SGW, D),
                      "w_br_diff": (DIFF_V, D), "w_o": (D, D), "w_up": (D, 2 * DFF), "w_down": (DFF, D)}
        self.wsh, self.wbs, self.wfull = {}, {}, {}
        for n, (K, N) in self.wspec.items():
            self.wsh[n] = nc.dram_tensor(n, [L, K // NC, N], F32, kind="ExternalInput")
            self.wfull[n] = [nc.dram_tensor(f"{n}_full{l}", [K, N], BF16) for l in range(L)]
            self.wbs[n] = ([nc.dram_tensor(f"{n}_bs{l}", [K // NC, N], BF16) for l in range(L)]
                           if NC > 1 else self.wfull[n])
        self.wgath = self.wfull
        self.p_gains = nc.dram_tensor("gains", [L, 128, 128], F32, kind="ExternalInput")
        self.p_glrw = nc.dram_tensor("glr_w", [L, 17, GLA_QK], F32, kind="ExternalInput")
        self.p_bc = nc.dram_tensor("bc", [L, 128, 192 + 256 + 1024 + 1024], F32, kind="ExternalInput")
        self.p_sgw = nc.dram_tensor("sgw", [L, 128, 1024], F32, kind="ExternalInput")
        self.p_sgb = nc.dram_tensor("sgb", [L, 128, 8], F32, kind="ExternalInput")
        self.p_lam = nc.dram_tensor("lam", [L, 128, 512], F32, kind="ExternalInput")
        self.p_conv = nc.dram_tensor("conv", [L, 128, 96 * 4], F32, kind="ExternalInput")
        self.p_consts = nc.dram_tensor("consts", [128, 128 + 128 + 96], F32, kind="ExternalInput")
        def sc(name, shape, dt):
            kind = "ExternalOutput" if name in self.dbg else "Internal"
            return nc.dram_tensor(name, list(shape), dt, kind=kind)
        self.XR = sc("XR", [D, S], F32)
        self.Y = sc("Y", [D, S], F32)
        self.QG = sc("QG", [GLA_QK, S], F32)
        self.KG = sc("KG", [GLA_QK, S], F32)
        self.LR = sc("LR", [GLA_R, S], F32)
        self.GV = sc("GV", [S, GLA_V], BF16)
        self.GG = sc("GG", [S, GLA_V], F32)
        self.SU = sc("SU", [S, SGW], F32)
        self.SV = sc("SV", [S, SGW], F32)
        self.DQ = sc("DQ", [DIFF_QK, S], BF16)
        self.DK = sc("DK", [DIFF_QK, S], BF16)
        self.DV = sc("DV", [S, DIFF_V], BF16)
        self.GT = sc("GT", [3 * D, S], BF16)
        self.OG = sc("OG", [GLA_V, S], BF16)
        self.OS = sc("OS", [SGW, S], BF16)
        self.OD = sc("OD", [DIFF_V, S], BF16)

    def consts(self):
        nc = self.nc
        a = lambda n, s, d: nc.alloc_sbuf_tensor(n, s, d)
        self.c_raw = a("c_raw", [128, 352], F32)
        self.mask_f = self.c_raw[:, 0:128]
        self.ident_f = self.c_raw[:, 128:256]
        self.alibi = self.c_raw[:, 256:352]
        self.c_b = a("c_b", [128, 384], BF16)
        self.mask_b = self.c_b[:, 0:128]
        self.ident_b = self.c_b[:, 128:256]
        self.ones_b = self.c_b[:, 256:384]
        d = self.ds()
        t = self.dma(self.c_raw[:, :], self.p_consts[:, :], d)
        self.DVE.wait(t)
        self.DVE.sig(nc.vector.tensor_copy(out=self.c_b[:, 0:256], in_=self.c_raw[:, 0:256]))
        self.DVE.sig(nc.vector.memset(self.c_b[:, 256:384], 1.0))

    def prep_setup(self):
        self.cast_ds = [DSem(self.nc.alloc_semaphore(f"castds{l}")) for l in range(self.L)]
        self.cc_ds = [DSem(self.nc.alloc_semaphore(f"ccds{l}")) for l in range(self.L)]

    def prep_cast(self, l):
        if l >= self.L:
            return
        NC, POOL = self.NC, self.POOL
        d = self.cast_ds[l]
        for n, (K, N) in self.wspec.items():
            R = K // NC
            src, dst = self.wsh[n], self.wbs[n][l]
            rstep = 512 if R > 512 else R
            if R % rstep:
                rstep = R // 2
            for r0 in range(0, R, rstep):
                for c0 in range(0, N, 4096):
                    c1 = min(N, c0 + 4096)
                    self.dma(dst[r0:r0 + rstep, c0:c1], src[l, r0:r0 + rstep, c0:c1], d, q=POOL)

    def prep_gather(self, l):
        if l >= self.L or self.NC == 1:
            return
        nc, NC, POOL = self.nc, self.NC, self.POOL
        POOL.wait(self.cast_ds[l].tok())
        cc = self.cc_ds[l]
        for n in self.wspec:
            nc.gpsimd.collective_compute("AllGather", ALU.bypass, replica_groups=[list(range(NC))],
                                         ins=[self.wbs[n][l].ap().opt()],
                                         outs=[self.wgath[n][l].ap().opt()]).then_inc(cc.sem, 1)
            cc.cnt += 1

    def prep_wait(self, l):
        t = self.cast_ds[l].tok() if self.NC == 1 else self.cc_ds[l].tok()
        self.SP.wait(t)
        self.POOL.wait(t)

    def phase_init(self):
        nc, S, PE, ACT, DVE = self.nc, self.S, self.PE, self.ACT, self.DVE
        with self.phase():
            self.prep_setup()
            self.prep_cast(0)
            self.prep_gather(0)
            self.prep_cast(1)
            xt = self.sb([128, 4, D], F32, "xin")
            psr = self.make_psring(4)
            stg = self.make_stage(4, [128, 512], F32)
            xd = self.ds()
            xwar = None
            for tg in range(S // 512):
                for b in range(4):
                    lt = self.dma(xt[:, b, :], self.x_in[tg * 512 + b * 128: tg * 512 + (b + 1) * 128, :], xd,
                                  waits=[xwar])
                PE.wait(lt)
                for kc in range(D // 128):
                    pi, pst, pwar = psr.next()
                    PE.wait(pwar)
                    for b in range(4):
                        ins = nc.tensor.transpose(out=pst[:, b * 128:(b + 1) * 128],
                                                  in_=xt[:, b, kc * 128:(kc + 1) * 128], identity=self.ident_f)
                    mt = PE.sig(ins)
                    psr.release(pi, self.evac_store(stg, pst[:, :], self.XR[kc * 128:(kc + 1) * 128, tg * 512:(tg + 1) * 512],
                                                    lambda t: t[:, :], mt))
                xwar = Tok(PE.sem, PE.cnt)

    def phase_final(self):
        nc, S, PE = self.nc, self.S, self.PE
        with self.phase():
            xt = self.sb([128, 32, 128], F32, "xfin")
            psr = self.make_psring(4)
            stg = self.make_stage(3, [128, 512], F32)
            xd = self.ds()
            xwar = None
            src = self.XR.ap().rearrange("(k p) s -> p k s", p=128)
            for tb in range(S // 128):
                for k0 in range(0, 32, 8):
                    lt = self.dma(xt[:, k0:k0 + 8, :], src[:, k0:k0 + 8, tb * 128:(tb + 1) * 128], xd, waits=[xwar])
                PE.wait(lt)
                for kg in range(8):
                    pi, pst, pwar = psr.next()
                    PE.wait(pwar)
                    for b in range(4):
                        ins = nc.tensor.transpose(out=pst[:, b * 128:(b + 1) * 128], in_=xt[:, kg * 4 + b, :],
                                                  identity=self.ident_f)
                    mt = PE.sig(ins)
                    psr.release(pi, self.evac_store(stg, pst[:, :],
                                                    self.y_out[tb * 128:(tb + 1) * 128, kg * 512:(kg + 1) * 512],
                                                    lambda t: t[:, :], mt))
                xwar = Tok(PE.sem, PE.cnt)

    def load_small(self, tile_ap, src_ap):
        d = self.ds()
        return self.dma(tile_ap, src_ap, d)

    def norm_to_hT(self, l, gcol0, t0, hT, hT_war, ctx):
        nc, DVE, TT = self.nc, self.DVE, self.TT
        xring, sqring, psn, rstd, g, gtok = ctx
        rt = self.norm_rstd(self.XR, t0, TT, xring, sqring, psn, self.ones_b, rstd)
        tok = None
        for kc in range(32):
            xi, xt, xwar = xring.next()
            lt = self.dma(xt[:, 0:TT], self.XR[kc * 128:(kc + 1) * 128, t0:t0 + TT], xring.dsem[xi], waits=[xwar])
            DVE.wait(lt, rt, hT_war, gtok)
            tok = DVE.sig(nc.vector.scalar_tensor_tensor(out=hT[:, kc, :], in0=xt[:, 0:TT],
                                                         scalar=g[:, gcol0 + kc:gcol0 + kc + 1], in1=rstd[:, 0:TT],
                                                         op0=ALU.mult, op1=ALU.mult))
            xring.release(xi, tok)
        self.rstd_war = tok
        return tok

    def norm_ctx(self, l, nx=3):
        TT = self.TT
        xring = self.make_xring(nx, TT)
        sqring = Ring([self.sb([128, TT], BF16, "sq") for _ in range(2)])
        psn = self.ps([128, 512], F32, "psn")
        rstd = self.sb([128, TT], F32, "rstd")
        g = self.sb([128, 128], F32, "gain")
        gtok = self.load_small(g[:, :], self.p_gains[l, :, :])
        self.psn_war = None
        self.rstd_war = None
        return (xring, sqring, psn, rstd, g, gtok)

    def phase_A(self, l):
        nc, S, TT, PE = self.nc, self.S, self.TT, self.PE
        W = self.wfull["w_in"][l]
        with self.phase():
            self.prep_wait(l)
            ctx = self.norm_ctx(l)
            wring = self.make_wring(3, 32 * 512)
            psring = self.make_psring(4)
            hT = self.sb([128, 32, TT], BF16, "hT")
            stF = self.make_stage(3, [128, 512], F32)
            stB = self.make_stage(3, [128, 512], BF16)
            hwar = None
            for tt in range(self.NTT):
                t0 = tt * TT
                htok = self.norm_to_hT(l, 0, t0, hT, hwar, ctx)
                PE.wait(htok)
                panels = []

                def seg(c0, width, form, dst, dt, func=None):
                    stage = stF if dt == F32 else stB
                    for p0 in range(0, width, 512):
                        w = min(512, width - p0)
                        if form == "F":
                            def ev(j, pst, tok, p0=p0, w=w):
                                m = min(128, w - j * 128)
                                r0 = p0 + j * 128
                                return self.evac_store(stage, pst[0:m, 0:TT], dst[r0:r0 + m, t0:t0 + TT],
                                                       lambda t: t[0:m, 0:TT], tok, func=func)
                        else:
                            def ev(mi, pst, tok, p0=p0, w=w):
                                r0 = t0 + mi * 128
                                return self.evac_store(stage, pst[:, 0:w], dst[r0:r0 + 128, p0:p0 + w],
                                                       lambda t: t[:, 0:w], tok, func=func)
                        panels.append(dict(W=W, KC=32, c0=c0 + p0, w=w, form=form, act=hT, evac=ev))

                seg(O_LR, GLA_R, "F", self.LR, F32)
                seg(O_GQ, GLA_QK, "F", self.QG, F32)
                seg(O_GK, GLA_QK, "F", self.KG, F32)
                seg(O_GV, GLA_V, "T", self.GV, BF16)
                seg(O_GG, GLA_V, "T", self.GG, F32, AF.Silu)
                seg(O_SU, SGW, "T", self.SU, F32)
                seg(O_SV, SGW, "T", self.SV, F32)
                seg(O_DQ, DIFF_QK, "F", self.DQ, BF16)
                seg(O_DK, DIFF_QK, "F", self.DK, BF16)
                seg(O_DV, DIFF_V, "T", self.DV, BF16)
                seg(O_GT, 3 * D, "F", self.GT, BF16, AF.Sigmoid)
                self.run_panels(wring, psring, panels, TT)
                hwar = Tok(PE.sem, PE.cnt)

    def phase_F(self, l, gcol0):
        nc, TT, DVE, POOL = self.nc, self.TT, self.DVE, self.POOL
        LOOK = 4
        with self.phase():
            ctx = self.norm_ctx(l, nx=6)
            xring, sqring, psn, rstd, g, gtok = ctx
            rx = self.make_xring(6, TT)
            for tt in range(self.NTT):
                t0 = tt * TT
                rt = self.norm_rstd(self.Y, t0, TT, xring, sqring, psn, self.ones_b, rstd)
                tok = None
                pend = []
                for kc in range(32 + LOOK):
                    if kc < 32:
                        yi, yt, ywar = xring.next()
                        ly = self.dma(yt[:, :], self.Y[kc * 128:(kc + 1) * 128, t0:t0 + TT], xring.dsem[yi], waits=[ywar])
                        xi, xt, xwar = rx.next()
                        lx = self.dma(xt[:, :], self.XR[kc * 128:(kc + 1) * 128, t0:t0 + TT], rx.dsem[xi], waits=[xwar])
                        pend.append((kc, yi, yt, ly, xi, xt, lx))
                    if kc >= LOOK:
                        k, yi, yt, ly, xi, xt, lx = pend.pop(0)
                        DVE.wait(ly, rt, gtok)
                        tok = DVE.sig(nc.vector.scalar_tensor_tensor(out=yt[:, :], in0=yt[:, :],
                                                                     scalar=g[:, gcol0 + k:gcol0 + k + 1], in1=rstd[:, :],
                                                                     op0=ALU.mult, op1=ALU.mult))
                        POOL.wait(tok, lx)
                        pt = POOL.sig(nc.gpsimd.tensor_tensor(out=xt[:, :], in0=xt[:, :], in1=yt[:, :], op=ALU.add))
                        xring.release(yi, pt)
                        st = self.dma(self.XR[k * 128:(k + 1) * 128, t0:t0 + TT], xt[:, :], rx.dsem[xi], waits=[pt])
                        rx.release(xi, st)
                self.rstd_war = tok

    def phase_E(self, l):
        nc, TT, PE, ACT, DVE, POOL, SP = self.nc, self.TT, self.PE, self.ACT, self.DVE, self.POOL, self.SP
        Wb = [self.wfull["w_br_gla"][l], self.wfull["w_br_sg"][l], self.wfull["w_br_diff"][l]]
        KCb = [12, 8, 12]
        k0b = [0, 12, 20]
        Wo = self.wfull["w_o"][l]
        with self.phase():
            wring = self.make_wring(5, 8192)
            psring = self.make_psring(6)
            act = self.sb([128, 32, TT], BF16, "actE")
            mrg = self.sb([128, 32, TT], BF16, "mrg")
            acc = self.sb([128, 4, TT], F32, "accE")
            acc_war = [None] * 4
            tring = Ring([self.sb([128, TT], F32, "tE") for _ in range(3)])
            gring = Ring([self.sb([128, 3, 4, TT], BF16, "gE") for _ in range(2)])
            gring.dsem = [self.ds() for _ in range(2)]
            stF = self.make_stage(3, [128, 512], F32)
            a_ds = self.ds()
            gsrc = self.GT.ap().rearrange("(b k p) s -> p b k s", b=3, p=128)
            act_war = None
            mrg_war = None
            for tt in range(self.NTT):
                t0 = tt * TT
                for src, k0, kc in ((self.OG, 0, 12), (self.OS, 12, 8), (self.OD, 20, 12)):
                    atok = self.dma(act[:, k0:k0 + kc, :], src.ap().rearrange("(k p) s -> p k s", p=128)[:, :, t0:t0 + TT],
                                    a_ds, waits=[act_war])
                PE.wait(atok)
                gtiles = {}

                def gload(pn):
                    gi, gt, gwar = gring.next()
                    tok = None
                    for b in range(3):
                        tok = self.dma(gt[:, b, :, :], gsrc[:, b, pn * 4:(pn + 1) * 4, t0:t0 + TT], gring.dsem[gi],
                                       waits=[gwar])
                    gtiles[pn] = (gi, gt, tok)

                gload(0)
                panels = []
                mtoks = []
                for pn in range(8):
                    for b in range(3):
                        def ev(j, pst, tok, pn=pn, b=b):
                            if b == 0 and j == 0 and pn + 1 < 8:
                                gload(pn + 1)
                            gi, gt, gtok = gtiles[pn]
                            if b == 0:
                                DVE.wait(tok, gtok, acc_war[j])
                                d = DVE.sig(nc.vector.tensor_tensor(out=acc[:, j, :], in0=pst[:, 0:TT], in1=gt[:, 0, j, :],
                                                                    op=ALU.mult))
                                acc_war[j] = d
                                return d
                            ti, tt_, twar = tring.next()
                            DVE.wait(tok, gtok, twar)
                            d = DVE.sig(nc.vector.tensor_tensor(out=tt_[:, :], in0=pst[:, 0:TT], in1=gt[:, b, j, :],
                                                                op=ALU.mult))
                            POOL.wait(d, acc_war[j])
                            if b == 1:
                                p = POOL.sig(nc.gpsimd.tensor_tensor(out=acc[:, j, :], in0=acc[:, j, :], in1=tt_[:, :],
                                                                     op=ALU.add))
                            else:
                                POOL.wait(mrg_war)
                                p = POOL.sig(nc.gpsimd.tensor_tensor(out=mrg[:, pn * 4 + j, :], in0=acc[:, j, :],
                                                                     in1=tt_[:, :], op=ALU.add))
                                mtoks.append(p)
                                if j == 3:
                                    gring.release(gi, p)
                            acc_war[j] = p
                            tring.release(ti, p)
                            return d
                        panels.append(dict(W=Wb[b], KC=KCb[b], c0=pn * 512, w=512, form="F",
                                           act=act[:, k0b[b]:k0b[b] + KCb[b], :], evac=ev))
                self.run_panels(wring, psring, panels, TT)
                act_war = Tok(PE.sem, PE.cnt)
                PE.wait(mtoks[-1])
                panels = []
                for pn in range(16):
                    def ev(j, pst, tok, pn=pn):
                        r0 = pn * 256 + j * 128
                        return self.evac_store(stF, pst[:, 0:TT], self.Y[r0:r0 + 128, t0:t0 + TT],
                                               lambda t: t[:, 0:TT], tok)
                    panels.append(dict(W=Wo, KC=32, c0=pn * 256, w=256, form="F", act=mrg, evac=ev))
                self.run_panels(wring, psring, panels, TT)
                mrg_war = Tok(PE.sem, PE.cnt)

    def phase_G(self, l):
        nc, TT, PE, ACT, DVE, POOL = self.nc, self.TT, self.PE, self.ACT, self.DVE, self.POOL
        Wu, Wd = self.wfull["w_up"][l], self.wfull["w_down"][l]
        with self.phase():
            ctx = self.norm_ctx(l)
            wring = self.make_wring(3, 12288)
            psring = self.make_psring(6)
            hT = self.sb([128, 32, TT], BF16, "hT")
            gT = self.sb([128, 48, TT], BF16, "gT")
            cw = self.sb([128, 96, 4], F32, "cw")
            cwt = self.load_small(cw[:, :, :], self.p_conv[l, :, :].rearrange("p (b j) -> p b j", j=4))
            halo = self.sb([128, 96, 2], F32, "halo")
            halo_tok = [None] * 96
            uring = Ring([self.sb([128, TT + 2], F32, "U") for _ in range(4)])
            aring = Ring([self.sb([128, TT], F32, "accA") for _ in range(5)])
            bring = Ring([self.sb([128, TT], F32, "accU") for _ in range(2)])
            stF = self.make_stage(3, [128, 512], F32)
            hwar = None
            gwar = None
            for tt in range(self.NTT):
                t0 = tt * TT
                htok = self.norm_to_hT(l, 64, t0, hT, hwar, ctx)
                PE.wait(htok)
                pend = {}

                def conv(blk, pst, tok, ring):
                    ui, U, uwar = uring.next()
                    ai, A, awar = ring.next()
                    POOL.wait(uwar, halo_tok[blk])
                    if tt == 0:
                        hin = POOL.sig(nc.gpsimd.memset(U[:, 0:2], 0.0))
                    else:
                        hin = POOL.sig(nc.gpsimd.tensor_copy(out=U[:, 0:2], in_=halo[:, blk, :]))
                    ACT.wait(tok, uwar, awar, cwt)
                    c1 = ACT.sig(nc.scalar.activation(out=U[:, 2:TT + 2], in_=pst[:, 0:TT], func=AF.Copy))
                    c2 = ACT.sig(nc.scalar.activation(out=A[:, :], in_=pst[:, 0:TT], func=AF.Identity,
                                                      scale=cw[:, blk, 2:3], bias=cw[:, blk, 3:4]))
                    POOL.wait(c1)
                    halo_tok[blk] = POOL.sig(nc.gpsimd.tensor_copy(out=halo[:, blk, :], in_=U[:, TT:TT + 2]))
                    DVE.wait(c1, c2, hin)
                    DVE.sig(nc.vector.scalar_tensor_tensor(out=A[:, :], in0=U[:, 1:TT + 1], scalar=cw[:, blk, 1:2],
                                                           in1=A[:, :], op0=ALU.mult, op1=ALU.add))
                    d = DVE.sig(nc.vector.scalar_tensor_tensor(out=A[:, :], in0=U[:, 0:TT], scalar=cw[:, blk, 0:1],
                                                               in1=A[:, :], op0=ALU.mult, op1=ALU.add))
                    uring.release(ui, halo_tok[blk] if False else d)
                    uring.war[ui] = [d, halo_tok[blk]]
                    return ai, A, d, c2

                panels = []
                for pr in range(16):
                    def ev_a(j, pst, tok, pr=pr):
                        blk = pr * 3 + j
                        ai, A, d, c2 = conv(blk, pst, tok, aring)
                        ACT.wait(d)
                        g = ACT.sig(nc.scalar.activation(out=A[:, :], in_=A[:, :], func=AF.Gelu_apprx_tanh))
                        pend[blk] = (ai, A, g)
                        return c2

                    def ev_u(j, pst, tok, pr=pr):
                        blk = pr * 3 + j
                        bi, B, d, c2 = conv(48 + blk, pst, tok, bring)
                        ai, A, g = pend.pop(blk)
                        DVE.wait(g, d, gwar)
                        m = DVE.sig(nc.vector.tensor_tensor(out=gT[:, blk, :], in0=A[:, :], in1=B[:, :], op=ALU.mult))
                        aring.release(ai, m)
                        bring.release(bi, m)
                        return c2
                    panels.append(dict(W=Wu, KC=32, c0=pr * 384, w=384, form="F", act=hT, evac=ev_a))
                    panels.append(dict(W=Wu, KC=32, c0=DFF + pr * 384, w=384, form="F", act=hT, evac=ev_u))
                self.run_panels(wring, psring, panels, TT)
                hwar = Tok(PE.sem, PE.cnt)
                PE.wait(Tok(DVE.sem, DVE.cnt))
                panels = []
                for pn in range(16):
                    def ev(j, pst, tok, pn=pn):
                        r0 = pn * 256 + j * 128
                        return self.evac_store(stF, pst[:, 0:TT], self.Y[r0:r0 + 128, t0:t0 + TT],
                                               lambda t: t[:, 0:TT], tok)
                    panels.append(dict(W=Wd, KC=48, c0=pn * 256, w=256, form="F", act=gT, evac=ev))
                self.run_panels(wring, psring, panels, TT)
                gwar = Tok(PE.sem, PE.cnt)

    def phase_B(self, l):
        nc, S, PE, ACT, DVE, POOL = self.nc, self.S, self.PE, self.ACT, self.DVE, self.POOL
        NCH = self.NCH
        H, DV = GLA_H, GLA_DV
        with self.phase():
            self.prep_gather(l + 1)
            self.prep_cast(l + 2)
            lra = self.sb([32, S], F32, "lra")
            waug = self.sb([32, GLA_QK], F32, "waug")
            POOL.sig(nc.gpsimd.memset(lra[:, :], 1.0))
            m1 = Tok(POOL.sem, POOL.cnt)
            lt1 = self.dma(lra[0:16, :], self.LR[:, :], self.ds(), waits=[m1])
            lt2 = self.load_small(waug[0:17, :], self.p_glrw[l, :, :])
            bc = self.sb([128, 192], F32, "gnbc")
            lt3 = self.load_small(bc[:, :], self.p_bc[l, :, 0:192])
            rmask = self.sb([128, 512], F32, "rmask")
            DVE.sig(nc.vector.memset(rmask[:, :], 1.0))
            DVE.wait(Tok(DVE.sem, DVE.cnt))
            for c in range(4):
                DVE.sig(nc.vector.memset(rmask[:, c * 128:c * 128 + 1], 0.0))
            rm_tok = Tok(DVE.sem, DVE.cnt)
            qh = self.sb([128, H, S], BF16, "qh")
            kd = self.sb([128, H, S], BF16, "kd")
            eb = self.sb([128, H, NCH], F32, "eb")
            psA = Ring([self.ps([128, 512], F32, "psA") for _ in range(2)])
            tA = Ring([self.sb([128, 512], F32, "tA") for _ in range(2)])
            tB = Ring([self.sb([128, 512], F32, "tB") for _ in range(2)])
            tC = Ring([self.sb([128, 512], F32, "tC") for _ in range(2)])
            tD = Ring([self.sb([128, 512], F32, "tD") for _ in range(2)])
            nb = Ring([self.sb([128, 8], F32, "nb") for _ in range(2)])
            qf = self.make_xring(2, 512)
            kf = self.make_xring(2, 512)
            PE.wait(lt1, lt2)
            scale = GLA_DK ** -0.5
            for h in range(H):
                for tg in range(S // 512):
                    t0 = tg * 512
                    pi, pst, pwar = psA.next()
                    PE.wait(pwar)
                    mt = PE.sig(nc.tensor.matmul(pst[:, :], lhsT=waug[0:17, h * 128:(h + 1) * 128],
                                                 rhs=lra[0:17, t0:t0 + 512], start=True, stop=True))
                    ai, A, awar = tA.next()
                    ACT.wait(mt, awar)
                    ACT.sig(nc.scalar.activation(out=A[:, :], in_=pst[:, :], func=AF.Exp, scale=-1.0))
                    a1 = Tok(ACT.sem, ACT.cnt)
                    psA.release(pi, a1)
                    ACT.wait(a1)
                    a2 = ACT.sig(nc.scalar.activation(out=A[:, :], in_=A[:, :], func=AF.Ln, bias=1.0))
                    bi, B, bwar = tB.next()
                    DVE.wait(a2, bwar, rm_tok)
                    d1 = DVE.sig(nc.vector.tensor_tensor_scan(out=B[:, :], data0=rmask[:, :], data1=A[:, :], initial=0.0,
                                                              op0=ALU.mult, op1=ALU.add))
                    tA.release(ai, d1)
                    Blast = B[:, :].rearrange("p (c t) -> p c t", t=128)[:, :, 127]
                    ni, NB, nwar = nb.next()
                    DVE.wait(d1, nwar)
                    DVE.sig(nc.vector.tensor_scalar(out=NB[:, 0:4], in0=Blast, scalar1=-1.0 / 16, scalar2=None,
                                                    op0=ALU.mult))
                    d2 = DVE.sig(nc.vector.tensor_scalar(out=NB[:, 4:8], in0=Blast, scalar1=1.0 / 16, scalar2=None,
                                                         op0=ALU.mult))
                    ACT.wait(d1, d2)
                    ACT.sig(nc.scalar.activation(out=eb[:, h, tg * 4:(tg + 1) * 4], in_=Blast, func=AF.Exp,
                                                 scale=-1.0 / 16))
                    ci, C, cwar = tC.next()
                    di, Dt, dwar = tD.next()
                    ACT.wait(cwar, dwar)
                    for c in range(4):
                        ACT.sig(nc.scalar.activation(out=C[:, c * 128:(c + 1) * 128], in_=B[:, c * 128:(c + 1) * 128],
                                                     func=AF.Exp, scale=1.0 / 16, bias=NB[:, c:c + 1]))
                        ACT.sig(nc.scalar.activation(out=Dt[:, c * 128:(c + 1) * 128], in_=B[:, c * 128:(c + 1) * 128],
                                                     func=AF.Exp, scale=-1.0 / 16, bias=NB[:, 4 + c:5 + c]))
                    a3 = Tok(ACT.sem, ACT.cnt)
                    tB.release(bi, a3)
                    nb.release(ni, a3)
                    qi, Q, qwar = qf.next()
                    lq = self.dma(Q[:, :], self.QG[h * 128:(h + 1) * 128, t0:t0 + 512], qf.dsem[qi], waits=[qwar])
                    ki, Kt, kwar = kf.next()
                    lk = self.dma(Kt[:, :], self.KG[h * 128:(h + 1) * 128, t0:t0 + 512], kf.dsem[ki], waits=[kwar])
                    DVE.wait(a3, lq)
                    d3 = DVE.sig(nc.vector.scalar_tensor_tensor(out=qh[:, h, t0:t0 + 512], in0=Q[:, :], scalar=scale,
                                                                in1=Dt[:, :], op0=ALU.mult, op1=ALU.mult))
                    qf.release(qi, d3)
                    tD.release(di, d3)
                    POOL.wait(a3, lk)
                    p3 = POOL.sig(nc.gpsimd.tensor_tensor(out=kd[:, h, t0:t0 + 512], in0=Kt[:, :], in1=C[:, :],
                                                          op=ALU.mult))
                    kf.release(ki, p3)
                    tC.release(ci, p3)
            pre_toks = [Tok(DVE.sem, DVE.cnt), Tok(POOL.sem, POOL.cnt), Tok(ACT.sem, ACT.cnt)]
            St = self.sb([128, H, DV], F32, "St")
            Sb = Ring([self.sb([128, DV], BF16, "Sb") for _ in range(3)])
            vr = Ring([self.sb([128, GLA_V], BF16, "vch") for _ in range(2)])
            vr.dsem = [self.ds() for _ in range(2)]
            gr = Ring([self.sb([128, GLA_V], F32, "sgg") for _ in range(2)])
            gr.dsem = [self.ds() for _ in range(2)]
            oraw = self.sb([128, H, DV], F32, "oraw")
            otmp = self.sb([128, H, DV], F32, "otmp")
            ogt = self.sb([128, GLA_V], BF16, "ogt")
            ss = self.sb([128, 8], F32, "ss")
            rs = self.sb([128, 8], F32, "rs")
            junk = self.sb([128, DV], F32, "junk")
            OGT = Ring([self.sb([128, 12, 512], BF16, "OGT") for _ in range(2)])
            OGT.dsem = [self.ds() for _ in range(2)]
            Ar = Ring([self.sb([128, 128], BF16, "A") for _ in range(3)])
            Kr = Ring([self.sb([128, 128], BF16, "kdt") for _ in range(3)])
            ps_s = Ring([self.ps([128, 512], F32, "ps_s") for _ in range(1)])
            ps_sub = Ring([ps_s.tiles[0][:, i * 128:(i + 1) * 128] for i in range(4)])
            ps_tT = self.ps([128, 1024], BF16, "ps_t")
            ps_t = Ring([ps_tT[:, i * 128:(i + 1) * 128] for i in range(4)])
            ps_o = Ring([self.ps([128, 512], F32, "ps_o") for _ in range(2)])
            ps_kv = Ring([self.ps([128, 512], F32, "ps_kv") for _ in range(1)])
            ps_kv = Ring([ps_kv.tiles[0][:, 0:DV], ps_kv.tiles[0][:, 256:256 + DV]])
            ps_TT = self.ps([128, 1024], BF16, "ps_T")
            PE.wait(*pre_toks)
            DVE.wait(*pre_toks)
            ACT.wait(*pre_toks)
            st_tok = [None] * H
            ss_war = None
            otmp_war = None
            oraw_war = None
            ogt_war = None
            psT_war = None
            gi_cur = None
            for n in range(NCH):
                c0 = n * 128
                vi, V, vwar = vr.next()
                lv = self.dma(V[:, :], self.GV[c0:c0 + 128, :], vr.dsem[vi], waits=[vwar])
                gi, G, gwar = gr.next()
                lg = self.dma(G[:, :], self.GG[c0:c0 + 128, :], gr.dsem[gi], waits=[gwar])
                PE.wait(lv)
                sstoks = []
                for h in range(H):
                    qc = qh[:, h, c0:c0 + 128]
                    kc_ = kd[:, h, c0:c0 + 128]
                    si, pss, swar = ps_sub.next()
                    PE.wait(swar)
                    m_s = PE.sig(nc.tensor.matmul(pss, lhsT=kc_, rhs=qc, start=True, stop=True))
                    ti, pst, twar = ps_t.next()
                    PE.wait(twar)
                    m_t = PE.sig(nc.tensor.transpose(out=pst, in_=kc_, identity=self.ident_b))
                    ai, A, awar = Ar.next()
                    DVE.wait(m_s, awar)
                    dA = DVE.sig(nc.vector.tensor_tensor(out=A[:, :], in0=pss, in1=self.mask_f, op=ALU.mult))
                    ps_sub.release(si, dA)
                    ki, KT, kwar = Kr.next()
                    ACT.wait(m_t, kwar)
                    aK = ACT.sig(nc.scalar.activation(out=KT[:, :], in_=pst, func=AF.Copy))
                    ps_t.release(ti, aK)
                    if n > 0:
                        DVE.wait(st_tok[h])
                        dS = DVE.sig(nc.vector.tensor_scalar(out=St[:, h, :], in0=St[:, h, :], scalar1=eb[:, h, n:n + 1],
                                                             scalar2=None, op0=ALU.mult))
                        bi, SB, bwar = Sb.next()
                        ACT.wait(dS, bwar)
                        aS = ACT.sig(nc.scalar.activation(out=SB[:, :], in_=St[:, h, :], func=AF.Copy))
                    oi, pso, owar = ps_o.next()
                    PE.wait(dA, owar)
                    m_o = PE.sig(nc.tensor.matmul(pso[:, 0:DV], lhsT=A[:, :], rhs=V[:, h * DV:(h + 1) * DV],
                                                  start=True, stop=(n == 0)))
                    if n > 0:
                        PE.wait(aS)
                        m_o = PE.sig(nc.tensor.matmul(pso[:, 0:DV], lhsT=qc, rhs=SB[:, :], start=False, stop=True))
                        Sb.release(bi, m_o)
                    Ar.release(ai, m_o)
                    vi2, psk, kvwar = ps_kv.next()
                    PE.wait(aK, kvwar)
                    m_k = PE.sig(nc.tensor.matmul(psk, lhsT=KT[:, :], rhs=V[:, h * DV:(h + 1) * DV], start=True, stop=True))
                    Kr.release(ki, m_k)
                    DVE.wait(m_k)
                    if n == 0:
                        dS2 = DVE.sig(nc.vector.tensor_copy(out=St[:, h, :], in_=psk))
                    else:
                        DVE.wait(aS)
                        dS2 = DVE.sig(nc.vector.tensor_tensor(out=St[:, h, :], in0=psk, in1=St[:, h, :], op=ALU.add))
                    st_tok[h] = dS2
                    ps_kv.release(vi2, dS2)
                    ACT.wait(m_o, oraw_war, ss_war)
                    ACT.sig(nc.scalar.activation(out=oraw[:, h, :], in_=pso[:, 0:DV], func=AF.Copy))
                    a_o = ACT.sig(nc.scalar.activation(out=junk[:, :], in_=pso[:, 0:DV], func=AF.Square,
                                                       accum_out=ss[:, h:h + 1]))
                    ps_o.release(oi, a_o)
                    sstoks.append(a_o)
                vr.release(vi, Tok(PE.sem, PE.cnt))
                ACT.wait(sstoks[-1], oraw_war)
                a_r = ACT.sig(nc.scalar.activation(out=rs[:, :], in_=ss[:, :], func=AF.Sqrt, scale=1.0 / DV, bias=EPS))
                DVE.wait(a_r, lt3)
                d_r = DVE.sig(nc.vector.reciprocal(out=rs[:, :], in_=rs[:, :]))
                ss_war = a_r
                DVE.wait(d_r, otmp_war)
                for h in range(H):
                    d_n = DVE.sig(nc.vector.scalar_tensor_tensor(out=otmp[:, h, :], in0=oraw[:, h, :], scalar=rs[:, h:h + 1],
                                                                 in1=bc[:, :], op0=ALU.mult, op1=ALU.mult))
                oraw_war = d_n
                POOL.wait(d_n, lg, ogt_war)
                p_g = POOL.sig(nc.gpsimd.tensor_tensor(out=ogt[:, :], in0=otmp[:, :, :].rearrange("p h d -> p (h d)"),
                                                       in1=G[:, :], op=ALU.mult))
                gr.release(gi, p_g)
                otmp_war = p_g
                if n % 4 == 0:
                    gi_cur = OGT.next()
                oi_, OT, otwar = gi_cur
                for f0, nf in ((0, 8), (8, 4)):
                    PE.wait(p_g, psT_war)
                    for f in range(nf):
                        ins = nc.tensor.transpose(out=ps_TT[:, f * 128:(f + 1) * 128],
                                                  in_=ogt[:, (f0 + f) * 128:(f0 + f + 1) * 128], identity=self.ident_b)
                    m_T = PE.sig(ins)
                    ACT.wait(m_T, otwar if n % 4 == 0 else None)
                    a_T = ACT.sig(nc.scalar.activation(
                        out=OT[:, f0:f0 + nf, (n % 4) * 128:(n % 4 + 1) * 128],
                        in_=ps_TT[:, 0:nf * 128].rearrange("p (f t) -> p f t", t=128), func=AF.Copy))
                    psT_war = a_T
                ogt_war = Tok(PE.sem, PE.cnt)
                if n % 4 == 3:
                    q0 = (n // 4) * 512
                    stt = self.dma(self.OG.ap().rearrange("(k p) s -> p k s", p=128)[:, :, q0:q0 + 512], OT[:, :, :],
                                   OGT.dsem[oi_], waits=[a_T])
                    OGT.release(oi_, stt)

    def phase_C(self, l):
        nc, S, PE, ACT, DVE, POOL = self.nc, self.S, self.PE, self.ACT, self.DVE, self.POOL
        with self.phase():
            lng = self.sb([128, 2048], F32, "lngb")
            t1 = self.load_small(lng[:, :], self.p_bc[l, :, 448:2496])
            wsf = self.sb([128, 8, 128], F32, "wsf")
            t2 = self.load_small(wsf[:, :, :], self.p_sgw[l, :, :].rearrange("p (g t) -> p g t", t=128))
            bs = self.sb([128, 8], F32, "bs")
            t3 = self.load_small(bs[:, :], self.p_sgb[l, :, :])
            wsb = self.sb([128, 8, 128], BF16, "wsb")
            DVE.wait(t2)
            for g in range(8):
                DVE.sig(nc.vector.tensor_tensor(out=wsb[:, g, :], in0=wsf[:, g, :], in1=self.mask_f, op=ALU.mult))
            w_tok = Tok(DVE.sem, DVE.cnt)
            ur = self.make_xring(2, 1024)
            vr = self.make_xring(2, 1024)
            gu = Ring([self.sb([128, 1024], F32, "gu") for _ in range(2)])
            gv = Ring([self.sb([128, 1024], F32, "gv") for _ in range(2)])
            vn = Ring([self.sb([128, 1024], BF16, "vn") for _ in range(2)])
            junk = self.sb([128, 1024], F32, "junkC")
            stat = Ring([self.sb([128, 4], F32, "stat") for _ in range(2)])
            osg = Ring([self.sb([128, 1024], BF16, "osg") for _ in range(2)])
            psf = Ring([self.ps([128, 1024], F32, "psf") for _ in range(2)])
            psT = Ring([self.ps([128, 1024], BF16, "psTC") for _ in range(2)])
            OST = Ring([self.sb([128, 8, 512], BF16, "OST") for _ in range(2)])
            OST.dsem = [self.ds() for _ in range(2)]
            PE.wait(w_tok)
            cur = None
            for n in range(self.NCH):
                c0 = n * 128
                ui, U, uwar = ur.next()
                lu = self.dma(U[:, :], self.SU[c0:c0 + 128, :], ur.dsem[ui], waits=[uwar])
                vi, V, vwar = vr.next()
                lv = self.dma(V[:, :], self.SV[c0:c0 + 128, :], vr.dsem[vi], waits=[vwar])
                gui, GU, guwar = gu.next()
                gvi, GV_, gvwar = gv.next()
                si, ST, swar = stat.next()
                ACT.wait(lu, lv, guwar, gvwar, swar)
                a1 = ACT.sig(nc.scalar.activation(out=GU[:, :], in_=U[:, :], func=AF.Gelu_apprx_tanh))
                ur.release(ui, a1)
                a2 = ACT.sig(nc.scalar.activation(out=GV_[:, :], in_=V[:, :], func=AF.Gelu_apprx_tanh,
                                                  accum_out=ST[:, 0:1]))
                vr.release(vi, a2)
                DVE.wait(a2)
                d1 = DVE.sig(nc.vector.tensor_scalar(out=ST[:, 1:2], in0=ST[:, 0:1], scalar1=1.0 / SGW, scalar2=None,
                                                     op0=ALU.mult))
                DVE.wait(d1)
                d2 = DVE.sig(nc.vector.tensor_scalar(out=GV_[:, :], in0=GV_[:, :], scalar1=ST[:, 1:2], scalar2=None,
                                                     op0=ALU.subtract))
                ACT.wait(d2)
                a3 = ACT.sig(nc.scalar.activation(out=junk[:, :], in_=GV_[:, :], func=AF.Square, accum_out=ST[:, 2:3]))
                ACT.wait(a3)
                a4 = ACT.sig(nc.scalar.activation(out=ST[:, 3:4], in_=ST[:, 2:3], func=AF.Sqrt, scale=1.0 / SGW, bias=EPS))
                DVE.wait(a4)
                d3 = DVE.sig(nc.vector.reciprocal(out=ST[:, 3:4], in_=ST[:, 3:4]))
                DVE.wait(d3, t1)
                d4 = DVE.sig(nc.vector.scalar_tensor_tensor(out=GV_[:, :], in0=GV_[:, :], scalar=ST[:, 3:4],
                                                            in1=lng[:, 0:1024], op0=ALU.mult, op1=ALU.mult))
                stat.release(si, d4)
                ni, VN, nwar = vn.next()
                POOL.wait(d4, nwar)
                p1 = POOL.sig(nc.gpsimd.tensor_tensor(out=VN[:, :], in0=GV_[:, :], in1=lng[:, 1024:2048], op=ALU.add))
                gv.release(gvi, p1)
                fi, PF, fwar = psf.next()
                PE.wait(p1, fwar)
                for g in range(8):
                    ins = nc.tensor.matmul(PF[:, g * 128:(g + 1) * 128], lhsT=wsb[:, g, :], rhs=VN[:, g * 128:(g + 1) * 128],
                                           start=True, stop=True)
                m1 = PE.sig(ins)
                vn.release(ni, m1)
                oi, OSG, owar = osg.next()
                DVE.wait(m1, a1, owar, t3)
                for g in range(8):
                    d5 = DVE.sig(nc.vector.scalar_tensor_tensor(out=OSG[:, g * 128:(g + 1) * 128],
                                                                in0=PF[:, g * 128:(g + 1) * 128], scalar=bs[:, g:g + 1],
                                                                in1=GU[:, g * 128:(g + 1) * 128], op0=ALU.add, op1=ALU.mult))
                psf.release(fi, d5)
                gu.release(gui, d5)
                ti, PT, twar = psT.next()
                PE.wait(d5, twar)
                for g in range(8):
                    ins = nc.tensor.transpose(out=PT[:, g * 128:(g + 1) * 128], in_=OSG[:, g * 128:(g + 1) * 128],
                                              identity=self.ident_b)
                m2 = PE.sig(ins)
                osg.release(oi, m2)
                if n % 4 == 0:
                    cur = OST.next()
                oi_, OT, otwar = cur
                ACT.wait(m2, otwar if n % 4 == 0 else None)
                a5 = ACT.sig(nc.scalar.activation(out=OT[:, :, (n % 4) * 128:(n % 4 + 1) * 128],
                                                  in_=PT[:, :].rearrange("p (f t) -> p f t", t=128), func=AF.Copy))
                psT.release(ti, a5)
                if n % 4 == 3:
                    q0 = (n // 4) * 512
                    stt = self.dma(self.OS.ap().rearrange("(k p) s -> p k s", p=128)[:, :, q0:q0 + 512], OT[:, :, :],
                                   OST.dsem[oi_], waits=[a5])
                    OST.release(oi_, stt)

    def phase_D(self, l):
        nc, S, PE, ACT, DVE, POOL = self.nc, self.S, self.PE, self.ACT, self.DVE, self.POOL
        NCH = self.NCH
        H, DV = DIFF_H, DIFF_DV
        lam_init = 0.8 - 0.6 * math.exp(-0.3 * l)
        with self.phase():
            lv = self.sb([128, 512], F32, "lamv")
            t1 = self.load_small(lv[:, :], self.p_lam[l, :, :])
            lw = self.sb([128, 8], F32, "lamw")
            junk = self.sb([128, 256], F32, "junkD")
            DVE.wait(t1)
            DVE.sig(nc.vector.tensor_tensor(out=junk[:, 0:128], in0=lv[:, 0:128], in1=lv[:, 128:256], op=ALU.mult))
            DVE.sig(nc.vector.tensor_tensor(out=junk[:, 128:256], in0=lv[:, 256:384], in1=lv[:, 384:512], op=ALU.mult))
            DVE.wait(Tok(DVE.sem, DVE.cnt))
            DVE.sig(nc.vector.reduce_sum(out=lw[:, 0:1], in_=junk[:, 0:128], axis=AX.X))
            d0 = DVE.sig(nc.vector.reduce_sum(out=lw[:, 1:2], in_=junk[:, 128:256], axis=AX.X))
            ACT.wait(d0)
            a0 = ACT.sig(nc.scalar.activation(out=lw[:, 2:4], in_=lw[:, 0:2], func=AF.Exp))
            DVE.wait(a0)
            DVE.sig(nc.vector.tensor_tensor(out=lw[:, 4:5], in0=lw[:, 2:3], in1=lw[:, 3:4], op=ALU.subtract))
            DVE.wait(Tok(DVE.sem, DVE.cnt))
            DVE.sig(nc.vector.tensor_scalar(out=lw[:, 5:6], in0=lw[:, 4:5], scalar1=lam_init, scalar2=-1.0,
                                            op0=ALU.add, op1=ALU.mult))
            dn = self.sb([128, 256], F32, "dnbc")
            t2 = self.load_small(dn[:, :], self.p_bc[l, :, 192:448])
            DVE.wait(t2, Tok(DVE.sem, DVE.cnt))
            DVE.sig(nc.vector.tensor_scalar(out=dn[:, :], in0=dn[:, :], scalar1=1.0 - lam_init, scalar2=None, op0=ALU.mult))
            lam_tok = Tok(DVE.sem, DVE.cnt)
            kT = self.sb([128, 12, S], BF16, "kT")
            kd_ = self.ds()
            ksrc = self.DK.ap().rearrange("(k p) s -> p k s", p=128)
            for k0 in range(0, 12, 4):
                ktok = self.dma(kT[:, k0:k0 + 4, :], ksrc[:, k0:k0 + 4, :], kd_)
            va = self.sb([128, NCH, H, DV + 1], BF16, "vaug")
            POOL.sig(nc.gpsimd.memset(va[:, :, :, DV:DV + 1], 1.0))
            vsrc = self.DV.ap().rearrange("(n p) (h d) -> p n h d", p=128, d=DV)
            vd_ = self.ds()
            for n in range(NCH):
                vtok = self.dma(va[:, n, :, 0:DV], vsrc[:, n, :, :], vd_)
            v_tok = [vtok, Tok(POOL.sem, POOL.cnt)]
            qT = Ring([self.sb([128, 12, 512], BF16, "qT") for _ in range(2)])
            qT.dsem = [self.ds() for _ in range(2)]
            qsrc = self.DQ.ap().rearrange("(k p) s -> p k s", p=128)
            Pr = Ring([self.sb([128, 128], BF16, "P") for _ in range(6)])
            O1 = self.sb([128, 4, DV], F32, "O1")
            o1_war = [None] * 4
            odt = self.sb([128, 4, DIFF_V], BF16, "odt")
            odt_war = None
            st = Ring([self.sb([128, 4], F32, "dst") for _ in range(4)])
            ps_s = Ring([self.ps([128, 512], F32, "ps_sD") for _ in range(2)])
            ps_o = [self.ps([128, 512], F32, "ps_oD") for _ in range(4)]
            pso_war = [None] * 4
            ps_TT = self.ps([128, 1024], BF16, "ps_TD")
            psT_war = None
            ODT = Ring([self.sb([128, 12, 512], BF16, "ODT") for _ in range(2)])
            ODT.dsem = [self.ds() for _ in range(2)]
            sc = DIFF_DK ** -0.5
            PE.wait(ktok, v_tok)
            for qg in range(S // 512):
                q0 = qg * 512
                qi, Q, qwar = qT.next()
                for k0 in range(0, 12, 4):
                    qtok = self.dma(Q[:, k0:k0 + 4, :], qsrc[:, k0:k0 + 4, q0:q0 + 512], qT.dsem[qi], waits=[qwar])
                PE.wait(qtok)
                for h in range(H):
                    for m in range(2):
                        hm = h * 2 + m
                        nkb = 4 * qg + 4
                        last_mm = [None] * 4
                        for j in range(nkb):
                            i_lo = max(j, 4 * qg)
                            nq = nkb - i_lo
                            si, pss, swar = ps_s.next()
                            PE.wait(swar)
                            m_s = PE.sig(nc.tensor.matmul(pss[:, 0:nq * 128], lhsT=kT[:, hm, j * 128:(j + 1) * 128],
                                                          rhs=Q[:, hm, (i_lo - 4 * qg) * 128:512], start=True, stop=True))
                            a_p = None
                            for i in range(i_lo, nkb):
                                ii = i - 4 * qg
                                pi, Pt, pwar = Pr.next()
                                ACT.wait(m_s, pwar)
                                a_p = ACT.sig(nc.scalar.activation(
                                    out=Pt[:, :], in_=pss[:, (i - i_lo) * 128:(i - i_lo + 1) * 128], func=AF.Exp, scale=sc,
                                    bias=self.alibi[:, h * 16 + (i - j):h * 16 + (i - j) + 1]))
                                rdy = a_p
                                if i == j:
                                    POOL.wait(a_p)
                                    rdy = POOL.sig(nc.gpsimd.tensor_tensor(out=Pt[:, :], in0=Pt[:, :], in1=self.mask_b,
                                                                           op=ALU.mult))
                                PE.wait(rdy)
                                if j == 0:
                                    PE.wait(pso_war[ii])
                                mm = PE.sig(nc.tensor.matmul(ps_o[ii][:, 0:DV + 1], lhsT=Pt[:, :], rhs=va[:, j, h, :],
                                                             start=(j == 0), stop=(j == i)))
                                Pr.release(pi, mm)
                                last_mm[ii] = mm
                            ps_s.release(si, a_p)
                        for ii in range(4):
                            sti, ST, stwar = st.next()
                            DVE.wait(last_mm[ii], stwar, lam_tok)
                            d1 = DVE.sig(nc.vector.reciprocal(out=ST[:, 0:1], in_=ps_o[ii][:, DV:DV + 1]))
                            if m == 0:
                                ACT.wait(d1, o1_war[ii])
                                a1 = ACT.sig(nc.scalar.activation(out=O1[:, ii, :], in_=ps_o[ii][:, 0:DV], func=AF.Copy,
                                                                  scale=ST[:, 0:1]))
                                pso_war[ii] = a1
                                st.release(sti, a1)
                                o1_war[ii] = a1
                            else:
                                DVE.wait(d1)
                                d2 = DVE.sig(nc.vector.tensor_scalar(out=ST[:, 1:2], in0=ST[:, 0:1], scalar1=lw[:, 5:6],
                                                                     scalar2=None, op0=ALU.mult))
                                DVE.wait(d2, o1_war[ii])
                                d3 = DVE.sig(nc.vector.scalar_tensor_tensor(out=O1[:, ii, :], in0=ps_o[ii][:, 0:DV],
                                                                            scalar=ST[:, 1:2], in1=O1[:, ii, :],
                                                                            op0=ALU.mult, op1=ALU.add))
                                pso_war[ii] = d3
                                ACT.wait(d3)
                                a2 = ACT.sig(nc.scalar.activation(out=junk[:, :], in_=O1[:, ii, :], func=AF.Square,
                                                                  accum_out=ST[:, 2:3]))
                                ACT.wait(a2)
                                a3 = ACT.sig(nc.scalar.activation(out=ST[:, 3:4], in_=ST[:, 2:3], func=AF.Sqrt,
                                                                  scale=1.0 / DV, bias=EPS))
                                DVE.wait(a3)
                                d4 = DVE.sig(nc.vector.reciprocal(out=ST[:, 3:4], in_=ST[:, 3:4]))
                                DVE.wait(d4, odt_war)
                                d5 = DVE.sig(nc.vector.scalar_tensor_tensor(out=odt[:, ii, h * DV:(h + 1) * DV],
                                                                            in0=O1[:, ii, :], scalar=ST[:, 3:4], in1=dn[:, :],
                                                                            op0=ALU.mult, op1=ALU.mult))
                                st.release(sti, d5)
                                o1_war[ii] = d5
                qT.release(qi, Tok(PE.sem, PE.cnt))
                oi_, OT, otwar = ODT.next()
                PE.wait(Tok(DVE.sem, DVE.cnt))
                a_T = None
                for ii in range(4):
                    for f0, nf in ((0, 8), (8, 4)):
                        PE.wait(psT_war)
                        for f in range(nf):
                            ins = nc.tensor.transpose(out=ps_TT[:, f * 128:(f + 1) * 128],
                                                      in_=odt[:, ii, (f0 + f) * 128:(f0 + f + 1) * 128],
                                                      identity=self.ident_b)
                        m_T = PE.sig(ins)
                        ACT.wait(m_T, otwar)
                        a_T = ACT.sig(nc.scalar.activation(
                            out=OT[:, f0:f0 + nf, ii * 128:(ii + 1) * 128],
                            in_=ps_TT[:, 0:nf * 128].rearrange("p (f t) -> p f t", t=128), func=AF.Copy))
                        psT_war = a_T
                odt_war = Tok(PE.sem, PE.cnt)
                stt = self.dma(self.OD.ap().rearrange("(k p) s -> p k s", p=128)[:, :, q0:q0 + 512], OT[:, :, :],
                               ODT.dsem[oi_], waits=[a_T])
                ODT.release(oi_, stt)

    def build(self, upto=None):
        self.declare()
        self.consts()
        self.phase_init()
        if upto == "init":
            return self.nc
        for l in range(self.L):
            for nm in ("A", "B", "C", "D", "E", "F1", "G", "F2"):
                if nm == "F1":
                    self.phase_F(l, 32)
                elif nm == "F2":
                    self.phase_F(l, 96)
                else:
                    getattr(self, "phase_" + nm)(l)
                if upto == (l, nm):
                    return self.nc
        self.phase_final()
        return self.nc


def host_small_params(inp, L):
    def gl(v):
        return np.asarray(v, np.float32).reshape(32, 128).T
    out = {}
    out["gains"] = np.stack([np.concatenate([gl(inp[k][l]) for k in ("g_pre_mix", "g_post_mix", "g_pre_ffn", "g_post_ffn")],
                                            axis=1) for l in range(L)]).astype(np.float32)
    out["glr_w"] = np.stack([np.concatenate([inp["w_gla_lr"][l], inp["b_gla_lr"][l][None]], axis=0)
                             for l in range(L)]).astype(np.float32)
    out["bc"] = np.stack([np.broadcast_to(np.concatenate([inp["gla_norm_g"][l], inp["diff_norm_g"][l], inp["sg_ln_g"][l],
                                                          inp["sg_ln_b"][l]])[None], (128, 2496))
                          for l in range(L)]).astype(np.float32)
    out["sgw"] = np.stack([np.asarray(inp["sg_w_s"][l]).transpose(2, 0, 1).reshape(128, 1024) for l in range(L)]).astype(np.float32)
    out["sgb"] = np.stack([np.asarray(inp["sg_b_s"][l]).T for l in range(L)]).astype(np.float32)
    out["lam"] = np.stack([np.broadcast_to(np.concatenate([inp["diff_lambda_q1"][l], inp["diff_lambda_k1"][l],
                                                           inp["diff_lambda_q2"][l], inp["diff_lambda_k2"][l]])[None],
                                           (128, 512)) for l in range(L)]).astype(np.float32)
    out["conv"] = np.stack([np.concatenate([inp["conv_w"][l], inp["conv_b"][l][None]], axis=0)
                            .reshape(4, 96, 128).transpose(2, 1, 0).reshape(128, 384) for l in range(L)]).astype(np.float32)
    idx = np.arange(128)
    mask = (idx[:, None] <= idx[None, :]).astype(np.float32)
    ident = np.eye(128, dtype=np.float32)
    sl = np.array(alibi_slopes(DIFF_H), np.float32)
    dist = np.arange(16)
    al = sl[None, :, None] * (idx[:, None, None] - 127.0 - dist[None, None, :] * 128.0)
    out["consts"] = np.concatenate([mask, ident, al.reshape(128, 96).astype(np.float32)], axis=1).astype(np.float32)
    return {k: np.ascontiguousarray(v) for k, v in out.items()}


WNAMES = ("w_in", "w_br_gla", "w_br_sg", "w_br_diff", "w_o", "w_up", "w_down")
_CACHE = {}


def kernel(**inputs):
    x = np.asarray(inputs["x"], np.float32)
    B, S, _ = x.shape
    L = inputs["w_in"].shape[0]
    NC = 8
    key = (S, L, NC)
    if key not in _CACHE:
        _CACHE[key] = Builder(S, L, NC).build()
    nc = _CACHE[key]
    small = host_small_params(inputs, L)
    in_maps = []
    for c in range(NC):
        m = {"x": np.ascontiguousarray(x[c])}
        for n in WNAMES:
            w = inputs[n]
            R = w.shape[1] // NC
            m[n] = np.ascontiguousarray(np.asarray(w[:, c * R:(c + 1) * R, :], np.float32))
        m.update(small)
        in_maps.append(m)
    res = run_bass_kernel_spmd(nc, in_maps, core_ids=list(range(NC)))
    return np.stack([res.results[c]["y"] for c in range(NC)], axis=0).astype(np.float32)
```

```python
import math
import contextlib
import numpy as np
import concourse.bass as bass
import concourse.mybir as mybir
from concourse.bass_utils import run_bass_kernel_spmd

F32 = mybir.dt.float32
BF16 = mybir.dt.bfloat16
AF = mybir.ActivationFunctionType
ALU = mybir.AluOpType
AX = mybir.AxisListType

D = 4096
GLA_H, GLA_DK, GLA_DV, GLA_R = 8, 128, 192, 16
SGW = 1024
DIFF_H, DIFF_DK, DIFF_DV = 6, 128, 256
DFF = 6144
EPS = 1e-6
GLA_QK = GLA_H * GLA_DK
GLA_V = GLA_H * GLA_DV
DIFF_QK = DIFF_H * 2 * DIFF_DK
DIFF_V = DIFF_H * DIFF_DV
SPLIT = (GLA_QK, GLA_QK, GLA_V, GLA_V, GLA_R, SGW, SGW, DIFF_QK, DIFF_QK, DIFF_V, 3 * D)
OFFS = [0]
for _s in SPLIT:
    OFFS.append(OFFS[-1] + _s)
IN_COLS = OFFS[-1]
O_GQ, O_GK, O_GV, O_GG, O_LR = OFFS[:5]
LR_PAD = 512 - GLA_R
O_SU, O_SV, O_DQ, O_DK, O_DV, O_GT = [o + LR_PAD for o in OFFS[5:11]]
IN_COLS_P = IN_COLS + LR_PAD
PANEL_W = {"w_in": 512, "w_br_gla": 512, "w_br_sg": 512, "w_br_diff": 512, "w_o": 256, "w_up": 384, "w_down": 256}


def alibi_slopes(n):
    def pow2(m):
        start = 2.0 ** (-8.0 / m)
        return [start ** (i + 1) for i in range(m)]
    if math.log2(n).is_integer():
        return pow2(n)
    p = 2 ** int(math.floor(math.log2(n)))
    return pow2(p) + pow2(2 * p)[0::2][: n - p]


class Tok:
    __slots__ = ("sem", "val")

    def __init__(self, sem, val):
        self.sem = sem
        self.val = val


class Eng:
    def __init__(self, nc, eng, name):
        self.eng = eng
        self.sem = nc.alloc_semaphore("sem_" + name)
        self.cnt = 0
        self.seen = {}

    def wait(self, *toks):
        for t in toks:
            if t is None:
                continue
            if isinstance(t, (list, tuple)):
                self.wait(*t)
                continue
            k = id(t.sem)
            if self.seen.get(k, 0) >= t.val:
                continue
            self.eng.wait_ge(t.sem, t.val)
            self.seen[k] = t.val

    def sig(self, ins):
        ins.then_inc(self.sem, 1)
        self.cnt += 1
        return Tok(self.sem, self.cnt)


class DSem:
    def __init__(self, sem):
        self.sem = sem
        self.cnt = 0

    def tok(self):
        return Tok(self.sem, self.cnt) if self.cnt else None


class Ring:
    def __init__(self, tiles):
        self.tiles = tiles
        self.war = [None] * len(tiles)
        self.k = 0

    def next(self):
        i = self.k % len(self.tiles)
        self.k += 1
        return i, self.tiles[i], self.war[i]

    def release(self, i, tok):
        self.war[i] = tok


class Builder:
    def __init__(self, S, L, NC, TT=512, dbg=()):
        self.S, self.L, self.NC, self.TT = S, L, NC, TT
        self.NTT = S // TT
        self.NCH = S // 128
        self.dbg = dbg
        self.serial_cc = False
        nc = self.nc = bass.Bass("TRN2", target_bir_lowering=False)
        self.PE = Eng(nc, nc.tensor, "pe")
        self.ACT = Eng(nc, nc.scalar, "act")
        self.DVE = Eng(nc, nc.vector, "dve")
        self.POOL = Eng(nc, nc.gpsimd, "pool")
        self.SP = Eng(nc, nc.sync, "sp")
        self.engs = [self.PE, self.ACT, self.DVE, self.POOL, self.SP]
        self.ds_free = []
        self.ds_used = []
        self.es = None
        self.uid = 0

    def name(self, p):
        self.uid += 1
        return f"{p}_{self.uid}"

    def ds(self):
        if self.ds_free:
            d = self.ds_free.pop()
        else:
            d = DSem(self.nc.alloc_semaphore(self.name("ds")))
        self.ds_used.append(d)
        return d

    def sb(self, shape, dtype, name="t"):
        return self.es.enter_context(self.nc.sbuf_tensor(self.name(name), list(shape), dtype))

    def ps(self, shape, dtype, name="p"):
        return self.es.enter_context(self.nc.psum_tensor(self.name(name), list(shape), dtype))

    def dma(self, out, in_, ds, waits=(), q=None):
        q = q or self.SP
        q.wait(*waits)
        q.eng.dma_start(out=out, in_=in_).then_inc(ds.sem, 16)
        ds.cnt += 16
        return Tok(ds.sem, ds.cnt)

    @contextlib.contextmanager
    def phase(self):
        with contextlib.ExitStack() as es:
            self.es = es
            yield
            self.phase_end()
        self.es = None

    def phase_end(self):
        nc = self.nc
        for d in self.ds_used:
            t = d.tok()
            if t is not None:
                (self.POOL if getattr(d, "pool", False) else self.SP).wait(t)
        nc.all_engine_barrier()
        for d in self.ds_used:
            if d.cnt:
                nc.sync.sem_clear(d.sem)
                d.cnt = 0
        for e in self.engs:
            if e.cnt:
                nc.sync.sem_clear(e.sem)
                e.cnt = 0
            e.seen = {}
        nc.all_engine_barrier()
        self.ds_free.extend(self.ds_used)
        self.ds_used = []

    def make_wring(self, nslots, elems):
        tiles = [self.sb([128, elems], BF16, "wr") for _ in range(nslots)]
        r = Ring(tiles)
        r.dsem = [self.ds() for _ in range(nslots)]
        return r

    def make_psring(self, n):
        return Ring([self.ps([128, 512], F32, "psg") for _ in range(n)])

    def run_panels(self, wring, psring, panels, ncols):
        nc, PE, SP = self.nc, self.PE, self.SP
        ns = len(wring.tiles)
        pre = ns - 1

        def issue(i):
            P = panels[i]
            s, tile, war = wring.next()
            SP.wait(war)
            KC, PW, pn = P["KC"], P["PW"], P["pn"]
            view = tile[:, 0:KC * PW].rearrange("p (k n) -> p k n", n=PW)
            tok = None
            half = (KC // 2) * PW
            for e0, e1 in ((0, half), (half, KC * PW)):
                tok = self.dma(tile[:, e0:e1], P["W"][pn * 128:(pn + 1) * 128, e0:e1], wring.dsem[s])
            P["slot"], P["view"], P["ltok"] = s, view, tok

        for i in range(min(pre, len(panels))):
            issue(i)
        for i, P in enumerate(panels):
            if i + pre < len(panels):
                issue(i + pre)
            PE.wait(P["ltok"])
            KC, w, view, act = P["KC"], P["w"], P["view"], P["act"]
            last = None
            if P["form"] == "F":
                nb = (w + 127) // 128
                for j in range(nb):
                    m = min(128, w - j * 128)
                    pi, pst, pwar = psring.next()
                    PE.wait(pwar)
                    for kc in range(KC):
                        ins = nc.tensor.matmul(pst[0:m, 0:ncols], lhsT=view[:, kc, j * 128:j * 128 + m],
                                               rhs=act[:, kc, 0:ncols], start=(kc == 0), stop=(kc == KC - 1))
                    last = PE.sig(ins)
                    psring.release(pi, P["evac"](j, pst, last))
            else:
                for mi in range(ncols // 128):
                    pi, pst, pwar = psring.next()
                    PE.wait(pwar)
                    for kc in range(KC):
                        ins = nc.tensor.matmul(pst[:, 0:w], lhsT=act[:, kc, mi * 128:(mi + 1) * 128],
                                               rhs=view[:, kc, :], start=(kc == 0), stop=(kc == KC - 1))
                    last = PE.sig(ins)
                    psring.release(pi, P["evac"](mi, pst, last))
            wring.release(P["slot"], last)

    def make_stage(self, n, shape, dtype):
        r = Ring([self.sb(shape, dtype, "stg") for _ in range(n)])
        r.dsem = [self.ds() for _ in range(n)]
        return r

    def evac_store(self, stage, src_ap, dst_ap, view, mm_tok, func=None, eng=None):
        si, st, _ = stage.next()
        war = stage.dsem[si].tok()
        if func is not None:
            eng = self.ACT
        if eng is None:
            eng = self.ACT if (stage.k % 2 == 0) else self.DVE
        eng.wait(mm_tok, war)
        o = view(st)
        if eng is self.ACT:
            ins = self.nc.scalar.activation(out=o, in_=src_ap, func=func or AF.Copy)
        else:
            ins = self.nc.vector.tensor_copy(out=o, in_=src_ap)
        tok = eng.sig(ins)
        self.dma(dst_ap, o, stage.dsem[si], waits=[tok])
        return tok

    def norm_rstd(self, src, t0, ncols, xring, sqring, psn, ones, rstd):
        nc, PE, ACT, DVE = self.nc, self.PE, self.ACT, self.DVE
        KC = D // 128
        ptok = None
        for kc in range(KC):
            xi, xt, xwar = xring.next()
            lt = self.dma(xt[:, 0:ncols], src[kc * 128:(kc + 1) * 128, t0:t0 + ncols], xring.dsem[xi], waits=[xwar])
            qi, qt, qwar = sqring.next()
            ACT.wait(lt, qwar)
            at = ACT.sig(nc.scalar.activation(out=qt[:, 0:ncols], in_=xt[:, 0:ncols], func=AF.Square))
            xring.release(xi, at)
            PE.wait(at)
            if kc == 0:
                PE.wait(self.psn_war)
            ptok = PE.sig(nc.tensor.matmul(psn[:, 0:ncols], lhsT=ones, rhs=qt[:, 0:ncols],
                                           start=(kc == 0), stop=(kc == KC - 1)))
            sqring.release(qi, ptok)
        ACT.wait(ptok, self.rstd_war)
        at = ACT.sig(nc.scalar.activation(out=rstd[:, 0:ncols], in_=psn[:, 0:ncols], func=AF.Sqrt,
                                          scale=1.0 / D, bias=EPS))
        self.psn_war = at
        DVE.wait(at)
        rt = DVE.sig(nc.vector.reciprocal(out=rstd[:, 0:ncols], in_=rstd[:, 0:ncols]))
        return rt

    def make_xring(self, n, ncols):
        r = Ring([self.sb([128, ncols], F32, "xr") for _ in range(n)])
        r.dsem = [self.ds() for _ in range(n)]
        return r

    def declare(self):
        nc, S, L, NC = self.nc, self.S, self.L, self.NC
        self.x_in = nc.dram_tensor("x", [S, D], F32, kind="ExternalInput")
        self.y_out = nc.dram_tensor("y", [S, D], F32, kind="ExternalOutput")
        self.wspec = {"w_in": (D, IN_COLS_P), "w_br_gla": (GLA_V, D), "w_br_sg": (SGW, D),
                      "w_br_diff": (DIFF_V, D), "w_o": (D, D), "w_up": (D, 2 * DFF), "w_down": (DFF, D)}
        self.wsh, self.wbs, self.wfull = {}, {}, {}
        for n, (K, N) in self.wspec.items():
            PW = PANEL_W[n]
            NP = N // PW
            NPc = NP // NC
            self.wsh[n] = nc.dram_tensor(n, [L, NPc, K, PW], F32, kind="ExternalInput")
            self.wfull[n] = [nc.dram_tensor(f"{n}_full{l}", [NP * 128, (K // 128) * PW], BF16) for l in range(L)]
            self.wbs[n] = ([nc.dram_tensor(f"{n}_bs{l}", [NPc * 128, (K // 128) * PW], BF16) for l in range(L)]
                           if NC > 1 else self.wfull[n])
        self.wgath = self.wfull
        self.p_gains = nc.dram_tensor("gains", [L, 128, 128], F32, kind="ExternalInput")
        self.p_glrw = nc.dram_tensor("glr_w", [L, 17, GLA_QK], F32, kind="ExternalInput")
        self.p_bc = nc.dram_tensor("bc", [L, 128, 192 + 256 + 1024 + 1024], F32, kind="ExternalInput")
        self.p_sgw = nc.dram_tensor("sgw", [L, 128, 1024], F32, kind="ExternalInput")
        self.p_sgb = nc.dram_tensor("sgb", [L, 128, 8], F32, kind="ExternalInput")
        self.p_lam = nc.dram_tensor("lam", [L, 128, 512], F32, kind="ExternalInput")
        self.p_conv = nc.dram_tensor("conv", [L, 128, 96 * 4], F32, kind="ExternalInput")
        self.p_consts = nc.dram_tensor("consts", [128, 128 + 128 + 96], F32, kind="ExternalInput")
        def sc(name, shape, dt):
            kind = "ExternalOutput" if name in self.dbg else "Internal"
            return nc.dram_tensor(name, list(shape), dt, kind=kind)
        self.XR = sc("XR", [D, S], F32)
        self.Y = sc("Y", [D, S], F32)
        self.QG = sc("QG", [GLA_QK, S], F32)
        self.KG = sc("KG", [GLA_QK, S], F32)
        self.LR = sc("LR", [GLA_R, S], F32)
        self.GV = sc("GV", [S, GLA_V], BF16)
        self.GG = sc("GG", [S, GLA_V], F32)
        self.SU = sc("SU", [S, SGW], F32)
        self.SV = sc("SV", [S, SGW], F32)
        self.DQ = sc("DQ", [DIFF_QK, S], BF16)
        self.DK = sc("DK", [DIFF_QK, S], BF16)
        self.DV = sc("DV", [S, DIFF_V], BF16)
        self.GT = sc("GT", [3 * D, S], BF16)
        self.OG = sc("OG", [GLA_V, S], BF16)
        self.OS = sc("OS", [SGW, S], BF16)
        self.OD = sc("OD", [DIFF_V, S], BF16)

    def consts(self):
        nc = self.nc
        a = lambda n, s, d: nc.alloc_sbuf_tensor(n, s, d)
        self.c_raw = a("c_raw", [128, 352], F32)
        self.mask_f = self.c_raw[:, 0:128]
        self.ident_f = self.c_raw[:, 128:256]
        self.alibi = self.c_raw[:, 256:352]
        self.c_b = a("c_b", [128, 384], BF16)
        self.mask_b = self.c_b[:, 0:128]
        self.ident_b = self.c_b[:, 128:256]
        self.ones_b = self.c_b[:, 256:384]
        d = self.ds()
        t = self.dma(self.c_raw[:, :], self.p_consts[:, :], d)
        self.DVE.wait(t)
        self.DVE.sig(nc.vector.tensor_copy(out=self.c_b[:, 0:256], in_=self.c_raw[:, 0:256]))
        self.DVE.sig(nc.vector.memset(self.c_b[:, 256:384], 1.0))

    def prep_setup(self):
        self.cast_ds = [DSem(self.nc.alloc_semaphore(f"castds{l}")) for l in range(self.L)]
        self.cc_ds = [DSem(self.nc.alloc_semaphore(f"ccds{l}")) for l in range(self.L)]

    def prep_cast(self, l):
        if l >= self.L:
            return
        NC, POOL = self.NC, self.POOL
        d = self.cast_ds[l]
        for n, (K, N) in self.wspec.items():
            PW = PANEL_W[n]
            NPc = (N // PW) // NC
            for q in range(NPc):
                src = self.wsh[n][l, q].rearrange("(kc p) n -> p kc n", p=128)
                dst = self.wbs[n][l][q * 128:(q + 1) * 128, :].rearrange("p (kc n) -> p kc n", n=PW)
                self.dma(dst, src, d, q=POOL)

    def prep_gather(self, l):
        if l >= self.L or self.NC == 1:
            return
        nc, NC, POOL = self.nc, self.NC, self.POOL
        POOL.wait(self.cast_ds[l].tok())
        cc = self.cc_ds[l]
        for n in self.wspec:
            nc.gpsimd.collective_compute("AllGather", ALU.bypass, replica_groups=[list(range(NC))],
                                         ins=[self.wbs[n][l].ap().opt()],
                                         outs=[self.wgath[n][l].ap().opt()]).then_inc(cc.sem, 1)
            cc.cnt += 1

    def prep_wait(self, l):
        t = self.cast_ds[l].tok() if self.NC == 1 else self.cc_ds[l].tok()
        self.SP.wait(t)
        self.POOL.wait(t)

    def phase_init(self):
        nc, S, PE, ACT, DVE = self.nc, self.S, self.PE, self.ACT, self.DVE
        with self.phase():
            self.prep_setup()
            self.prep_cast(0)
            self.prep_gather(0)
            self.prep_cast(1)
            xt = self.sb([128, 4, D], F32, "xin")
            psr = self.make_psring(4)
            stg = self.make_stage(4, [128, 512], F32)
            xd = self.ds()
            xwar = None
            for tg in range(S // 512):
                for b in range(4):
                    lt = self.dma(xt[:, b, :], self.x_in[tg * 512 + b * 128: tg * 512 + (b + 1) * 128, :], xd,
                                  waits=[xwar])
                PE.wait(lt)
                for kc in range(D // 128):
                    pi, pst, pwar = psr.next()
                    PE.wait(pwar)
                    for b in range(4):
                        ins = nc.tensor.transpose(out=pst[:, b * 128:(b + 1) * 128],
                                                  in_=xt[:, b, kc * 128:(kc + 1) * 128], identity=self.ident_f)
                    mt = PE.sig(ins)
                    psr.release(pi, self.evac_store(stg, pst[:, :], self.XR[kc * 128:(kc + 1) * 128, tg * 512:(tg + 1) * 512],
                                                    lambda t: t[:, :], mt))
                xwar = Tok(PE.sem, PE.cnt)

    def phase_final(self):
        nc, S, PE = self.nc, self.S, self.PE
        with self.phase():
            xt = self.sb([128, 32, 128], F32, "xfin")
            psr = self.make_psring(4)
            stg = self.make_stage(3, [128, 512], F32)
            xd = self.ds()
            xwar = None
            src = self.XR.ap().rearrange("(k p) s -> p k s", p=128)
            for tb in range(S // 128):
                for k0 in range(0, 32, 8):
                    lt = self.dma(xt[:, k0:k0 + 8, :], src[:, k0:k0 + 8, tb * 128:(tb + 1) * 128], xd, waits=[xwar])
                PE.wait(lt)
                for kg in range(8):
                    pi, pst, pwar = psr.next()
                    PE.wait(pwar)
                    for b in range(4):
                        ins = nc.tensor.transpose(out=pst[:, b * 128:(b + 1) * 128], in_=xt[:, kg * 4 + b, :],
                                                  identity=self.ident_f)
                    mt = PE.sig(ins)
                    psr.release(pi, self.evac_store(stg, pst[:, :],
                                                    self.y_out[tb * 128:(tb + 1) * 128, kg * 512:(kg + 1) * 512],
                                                    lambda t: t[:, :], mt))
                xwar = Tok(PE.sem, PE.cnt)

    def load_small(self, tile_ap, src_ap):
        d = self.ds()
        return self.dma(tile_ap, src_ap, d)

    def norm_to_hT(self, l, gcol0, t0, hT, hT_war, ctx):
        nc, DVE, TT = self.nc, self.DVE, self.TT
        xring, sqring, psn, rstd, g, gtok = ctx
        rt = self.norm_rstd(self.XR, t0, TT, xring, sqring, psn, self.ones_b, rstd)
        tok = None
        for kc in range(32):
            xi, xt, xwar = xring.next()
            lt = self.dma(xt[:, 0:TT], self.XR[kc * 128:(kc + 1) * 128, t0:t0 + TT], xring.dsem[xi], waits=[xwar])
            DVE.wait(lt, rt, hT_war, gtok)
            tok = DVE.sig(nc.vector.scalar_tensor_tensor(out=hT[:, kc, :], in0=xt[:, 0:TT],
                                                         scalar=g[:, gcol0 + kc:gcol0 + kc + 1], in1=rstd[:, 0:TT],
                                                         op0=ALU.mult, op1=ALU.mult))
            xring.release(xi, tok)
        self.rstd_war = tok
        return tok

    def norm_ctx(self, l, nx=3):
        TT = self.TT
        xring = self.make_xring(nx, TT)
        sqring = Ring([self.sb([128, TT], BF16, "sq") for _ in range(2)])
        psn = self.ps([128, 512], F32, "psn")
        rstd = self.sb([128, TT], F32, "rstd")
        g = self.sb([128, 128], F32, "gain")
        gtok = self.load_small(g[:, :], self.p_gains[l, :, :])
        self.psn_war = None
        self.rstd_war = None
        return (xring, sqring, psn, rstd, g, gtok)

    def phase_A(self, l):
        nc, S, TT, PE = self.nc, self.S, self.TT, self.PE
        W = self.wfull["w_in"][l]
        with self.phase():
            self.prep_wait(l)
            ctx = self.norm_ctx(l)
            wring = self.make_wring(3, 32 * 512)
            psring = self.make_psring(4)
            hT = self.sb([128, 32, TT], BF16, "hT")
            stF = self.make_stage(3, [128, 512], F32)
            stB = self.make_stage(3, [128, 512], BF16)
            hwar = None
            for tt in range(self.NTT):
                t0 = tt * TT
                htok = self.norm_to_hT(l, 0, t0, hT, hwar, ctx)
                PE.wait(htok)
                panels = []

                def seg(c0, width, form, dst, dt, func=None):
                    stage = stF if dt == F32 else stB
                    for p0 in range(0, width, 512):
                        w = min(512, width - p0)
                        if form == "F":
                            def ev(j, pst, tok, p0=p0, w=w):
                                m = min(128, w - j * 128)
                                r0 = p0 + j * 128
                                return self.evac_store(stage, pst[0:m, 0:TT], dst[r0:r0 + m, t0:t0 + TT],
                                                       lambda t: t[0:m, 0:TT], tok, func=func)
                        else:
                            def ev(mi, pst, tok, p0=p0, w=w):
                                r0 = t0 + mi * 128
                                return self.evac_store(stage, pst[:, 0:w], dst[r0:r0 + 128, p0:p0 + w],
                                                       lambda t: t[:, 0:w], tok, func=func)
                        panels.append(dict(W=W, KC=32, pn=(c0 + p0) // 512, PW=512, w=w, form=form, act=hT, evac=ev))

                seg(O_LR, GLA_R, "F", self.LR, F32)
                seg(O_GQ, GLA_QK, "F", self.QG, F32)
                seg(O_GK, GLA_QK, "F", self.KG, F32)
                seg(O_GV, GLA_V, "T", self.GV, BF16)
                seg(O_GG, GLA_V, "T", self.GG, F32, AF.Silu)
                seg(O_SU, SGW, "T", self.SU, F32)
                seg(O_SV, SGW, "T", self.SV, F32)
                seg(O_DQ, DIFF_QK, "F", self.DQ, BF16)
                seg(O_DK, DIFF_QK, "F", self.DK, BF16)
                seg(O_DV, DIFF_V, "T", self.DV, BF16)
                seg(O_GT, 3 * D, "F", self.GT, BF16, AF.Sigmoid)
                self.run_panels(wring, psring, panels, TT)
                hwar = Tok(PE.sem, PE.cnt)

    def phase_F(self, l, gcol0):
        nc, TT, DVE, POOL = self.nc, self.TT, self.DVE, self.POOL
        LOOK = 4
        with self.phase():
            ctx = self.norm_ctx(l, nx=6)
            xring, sqring, psn, rstd, g, gtok = ctx
            rx = self.make_xring(6, TT)
            for tt in range(self.NTT):
                t0 = tt * TT
                rt = self.norm_rstd(self.Y, t0, TT, xring, sqring, psn, self.ones_b, rstd)
                tok = None
                pend = []
                for kc in range(32 + LOOK):
                    if kc < 32:
                        yi, yt, ywar = xring.next()
                        ly = self.dma(yt[:, :], self.Y[kc * 128:(kc + 1) * 128, t0:t0 + TT], xring.dsem[yi], waits=[ywar])
                        xi, xt, xwar = rx.next()
                        lx = self.dma(xt[:, :], self.XR[kc * 128:(kc + 1) * 128, t0:t0 + TT], rx.dsem[xi], waits=[xwar])
                        pend.append((kc, yi, yt, ly, xi, xt, lx))
                    if kc >= LOOK:
                        k, yi, yt, ly, xi, xt, lx = pend.pop(0)
                        DVE.wait(ly, rt, gtok)
                        tok = DVE.sig(nc.vector.scalar_tensor_tensor(out=yt[:, :], in0=yt[:, :],
                                                                     scalar=g[:, gcol0 + k:gcol0 + k + 1], in1=rstd[:, :],
                                                                     op0=ALU.mult, op1=ALU.mult))
                        POOL.wait(tok, lx)
                        pt = POOL.sig(nc.gpsimd.tensor_tensor(out=xt[:, :], in0=xt[:, :], in1=yt[:, :], op=ALU.add))
                        xring.release(yi, pt)
                        st = self.dma(self.XR[k * 128:(k + 1) * 128, t0:t0 + TT], xt[:, :], rx.dsem[xi], waits=[pt])
                        rx.release(xi, st)
                self.rstd_war = tok

    def phase_E(self, l):
        nc, TT, PE, ACT, DVE, POOL, SP = self.nc, self.TT, self.PE, self.ACT, self.DVE, self.POOL, self.SP
        Wb = [self.wfull["w_br_gla"][l], self.wfull["w_br_sg"][l], self.wfull["w_br_diff"][l]]
        KCb = [12, 8, 12]
        k0b = [0, 12, 20]
        Wo = self.wfull["w_o"][l]
        with self.phase():
            wring = self.make_wring(5, 8192)
            psring = self.make_psring(6)
            act = self.sb([128, 32, TT], BF16, "actE")
            mrg = self.sb([128, 32, TT], BF16, "mrg")
            acc = self.sb([128, 4, TT], F32, "accE")
            acc_war = [None] * 4
            tring = Ring([self.sb([128, TT], F32, "tE") for _ in range(3)])
            gring = Ring([self.sb([128, 3, 4, TT], BF16, "gE") for _ in range(2)])
            gring.dsem = [self.ds() for _ in range(2)]
            stF = self.make_stage(3, [128, 512], F32)
            a_ds = self.ds()
            gsrc = self.GT.ap().rearrange("(b k p) s -> p b k s", b=3, p=128)
            act_war = None
            mrg_war = None
            for tt in range(self.NTT):
                t0 = tt * TT
                for src, k0, kc in ((self.OG, 0, 12), (self.OS, 12, 8), (self.OD, 20, 12)):
                    atok = self.dma(act[:, k0:k0 + kc, :], src.ap().rearrange("(k p) s -> p k s", p=128)[:, :, t0:t0 + TT],
                                    a_ds, waits=[act_war])
                PE.wait(atok)
                gtiles = {}

                def gload(pn):
                    gi, gt, gwar = gring.next()
                    tok = None
                    for b in range(3):
                        tok = self.dma(gt[:, b, :, :], gsrc[:, b, pn * 4:(pn + 1) * 4, t0:t0 + TT], gring.dsem[gi],
                                       waits=[gwar])
                    gtiles[pn] = (gi, gt, tok)

                gload(0)
                panels = []
                mtoks = []
                for pn in range(8):
                    for b in range(3):
                        def ev(j, pst, tok, pn=pn, b=b):
                            if b == 0 and j == 0 and pn + 1 < 8:
                                gload(pn + 1)
                            gi, gt, gtok = gtiles[pn]
                            if b == 0:
                                DVE.wait(tok, gtok, acc_war[j])
                                d = DVE.sig(nc.vector.tensor_tensor(out=acc[:, j, :], in0=pst[:, 0:TT], in1=gt[:, 0, j, :],
                                                                    op=ALU.mult))
                                acc_war[j] = d
                                return d
                            ti, tt_, twar = tring.next()
                            DVE.wait(tok, gtok, twar)
                            d = DVE.sig(nc.vector.tensor_tensor(out=tt_[:, :], in0=pst[:, 0:TT], in1=gt[:, b, j, :],
                                                                op=ALU.mult))
                            POOL.wait(d, acc_war[j])
                            if b == 1:
                                p = POOL.sig(nc.gpsimd.tensor_tensor(out=acc[:, j, :], in0=acc[:, j, :], in1=tt_[:, :],
                                                                     op=ALU.add))
                            else:
                                POOL.wait(mrg_war)
                                p = POOL.sig(nc.gpsimd.tensor_tensor(out=mrg[:, pn * 4 + j, :], in0=acc[:, j, :],
                                                                     in1=tt_[:, :], op=ALU.add))
                                mtoks.append(p)
                                if j == 3:
                                    gring.release(gi, p)
                            acc_war[j] = p
                            tring.release(ti, p)
                            return d
                        panels.append(dict(W=Wb[b], KC=KCb[b], pn=pn, PW=512, w=512, form="F",
                                           act=act[:, k0b[b]:k0b[b] + KCb[b], :], evac=ev))
                self.run_panels(wring, psring, panels, TT)
                act_war = Tok(PE.sem, PE.cnt)
                PE.wait(mtoks[-1])
                panels = []
                for pn in range(16):
                    def ev(j, pst, tok, pn=pn):
                        r0 = pn * 256 + j * 128
                        return self.evac_store(stF, pst[:, 0:TT], self.Y[r0:r0 + 128, t0:t0 + TT],
                                               lambda t: t[:, 0:TT], tok)
                    panels.append(dict(W=Wo, KC=32, pn=pn, PW=256, w=256, form="F", act=mrg, evac=ev))
                self.run_panels(wring, psring, panels, TT)
                mrg_war = Tok(PE.sem, PE.cnt)

    def phase_G(self, l):
        nc, TT, PE, ACT, DVE, POOL = self.nc, self.TT, self.PE, self.ACT, self.DVE, self.POOL
        Wu, Wd = self.wfull["w_up"][l], self.wfull["w_down"][l]
        with self.phase():
            ctx = self.norm_ctx(l)
            wring = self.make_wring(3, 12288)
            psring = self.make_psring(6)
            hT = self.sb([128, 32, TT], BF16, "hT")
            gT = self.sb([128, 48, TT], BF16, "gT")
            cw = self.sb([128, 96, 4], F32, "cw")
            cwt = self.load_small(cw[:, :, :], self.p_conv[l, :, :].rearrange("p (b j) -> p b j", j=4))
            halo = self.sb([128, 96, 2], F32, "halo")
            halo_tok = [None] * 96
            uring = Ring([self.sb([128, TT + 2], F32, "U") for _ in range(4)])
            aring = Ring([self.sb([128, TT], F32, "accA") for _ in range(5)])
            bring = Ring([self.sb([128, TT], F32, "accU") for _ in range(2)])
            stF = self.make_stage(3, [128, 512], F32)
            hwar = None
            gwar = None
            for tt in range(self.NTT):
                t0 = tt * TT
                htok = self.norm_to_hT(l, 64, t0, hT, hwar, ctx)
                PE.wait(htok)
                pend = {}

                def conv(blk, pst, tok, ring):
                    ui, U, uwar = uring.next()
                    ai, A, awar = ring.next()
                    POOL.wait(uwar, halo_tok[blk])
                    if tt == 0:
                        hin = POOL.sig(nc.gpsimd.memset(U[:, 0:2], 0.0))
                    else:
                        hin = POOL.sig(nc.gpsimd.tensor_copy(out=U[:, 0:2], in_=halo[:, blk, :]))
                    ACT.wait(tok, uwar, awar, cwt)
                    c1 = ACT.sig(nc.scalar.activation(out=U[:, 2:TT + 2], in_=pst[:, 0:TT], func=AF.Copy))
                    c2 = ACT.sig(nc.scalar.activation(out=A[:, :], in_=pst[:, 0:TT], func=AF.Identity,
                                                      scale=cw[:, blk, 2:3], bias=cw[:, blk, 3:4]))
                    POOL.wait(c1)
                    halo_tok[blk] = POOL.sig(nc.gpsimd.tensor_copy(out=halo[:, blk, :], in_=U[:, TT:TT + 2]))
                    DVE.wait(c1, c2, hin)
                    DVE.sig(nc.vector.scalar_tensor_tensor(out=A[:, :], in0=U[:, 1:TT + 1], scalar=cw[:, blk, 1:2],
                                                           in1=A[:, :], op0=ALU.mult, op1=ALU.add))
                    d = DVE.sig(nc.vector.scalar_tensor_tensor(out=A[:, :], in0=U[:, 0:TT], scalar=cw[:, blk, 0:1],
                                                               in1=A[:, :], op0=ALU.mult, op1=ALU.add))
                    uring.release(ui, halo_tok[blk] if False else d)
                    uring.war[ui] = [d, halo_tok[blk]]
                    return ai, A, d, c2

                panels = []
                for pr in range(16):
                    def ev_a(j, pst, tok, pr=pr):
                        blk = pr * 3 + j
                        ai, A, d, c2 = conv(blk, pst, tok, aring)
                        ACT.wait(d)
                        g = ACT.sig(nc.scalar.activation(out=A[:, :], in_=A[:, :], func=AF.Gelu_apprx_tanh))
                        pend[blk] = (ai, A, g)
                        return c2

                    def ev_u(j, pst, tok, pr=pr):
                        blk = pr * 3 + j
                        bi, B, d, c2 = conv(48 + blk, pst, tok, bring)
                        ai, A, g = pend.pop(blk)
                        DVE.wait(g, d, gwar)
                        m = DVE.sig(nc.vector.tensor_tensor(out=gT[:, blk, :], in0=A[:, :], in1=B[:, :], op=ALU.mult))
                        aring.release(ai, m)
                        bring.release(bi, m)
                        return c2
                    panels.append(dict(W=Wu, KC=32, pn=pr, PW=384, w=384, form="F", act=hT, evac=ev_a))
                    panels.append(dict(W=Wu, KC=32, pn=16 + pr, PW=384, w=384, form="F", act=hT, evac=ev_u))
                self.run_panels(wring, psring, panels, TT)
                hwar = Tok(PE.sem, PE.cnt)
                PE.wait(Tok(DVE.sem, DVE.cnt))
                panels = []
                for pn in range(16):
                    def ev(j, pst, tok, pn=pn):
                        r0 = pn * 256 + j * 128
                        return self.evac_store(stF, pst[:, 0:TT], self.Y[r0:r0 + 128, t0:t0 + TT],
                                               lambda t: t[:, 0:TT], tok)
                    panels.append(dict(W=Wd, KC=48, pn=pn, PW=256, w=256, form="F", act=gT, evac=ev))
                self.run_panels(wring, psring, panels, TT)
                gwar = Tok(PE.sem, PE.cnt)

    def phase_B(self, l):
        nc, S, PE, ACT, DVE, POOL = self.nc, self.S, self.PE, self.ACT, self.DVE, self.POOL
        NCH = self.NCH
        H, DV = GLA_H, GLA_DV
        with self.phase():
            self.prep_gather(l + 1)
            self.prep_cast(l + 2)
            lra = self.sb([32, S], F32, "lra")
            waug = self.sb([32, GLA_QK], F32, "waug")
            POOL.sig(nc.gpsimd.memset(lra[:, :], 1.0))
            m1 = Tok(POOL.sem, POOL.cnt)
            lt1 = self.dma(lra[0:16, :], self.LR[:, :], self.ds(), waits=[m1])
            lt2 = self.load_small(waug[0:17, :], self.p_glrw[l, :, :])
            bc = self.sb([128, 192], F32, "gnbc")
            lt3 = self.load_small(bc[:, :], self.p_bc[l, :, 0:192])
            rmask = self.sb([128, 512], F32, "rmask")
            DVE.sig(nc.vector.memset(rmask[:, :], 1.0))
            DVE.wait(Tok(DVE.sem, DVE.cnt))
            for c in range(4):
                DVE.sig(nc.vector.memset(rmask[:, c * 128:c * 128 + 1], 0.0))
            rm_tok = Tok(DVE.sem, DVE.cnt)
            qh = self.sb([128, H, S], BF16, "qh")
            kd = self.sb([128, H, S], BF16, "kd")
            eb = self.sb([128, H, NCH], F32, "eb")
            psA = Ring([self.ps([128, 512], F32, "psA") for _ in range(2)])
            tA = Ring([self.sb([128, 512], F32, "tA") for _ in range(2)])
            tB = Ring([self.sb([128, 512], F32, "tB") for _ in range(2)])
            tC = Ring([self.sb([128, 512], F32, "tC") for _ in range(2)])
            tD = Ring([self.sb([128, 512], F32, "tD") for _ in range(2)])
            nb = Ring([self.sb([128, 8], F32, "nb") for _ in range(2)])
            qf = self.make_xring(2, 512)
            kf = self.make_xring(2, 512)
            PE.wait(lt1, lt2)
            scale = GLA_DK ** -0.5
            for h in range(H):
                for tg in range(S // 512):
                    t0 = tg * 512
                    pi, pst, pwar = psA.next()
                    PE.wait(pwar)
                    mt = PE.sig(nc.tensor.matmul(pst[:, :], lhsT=waug[0:17, h * 128:(h + 1) * 128],
                                                 rhs=lra[0:17, t0:t0 + 512], start=True, stop=True))
                    ai, A, awar = tA.next()
                    ACT.wait(mt, awar)
                    ACT.sig(nc.scalar.activation(out=A[:, :], in_=pst[:, :], func=AF.Exp, scale=-1.0))
                    a1 = Tok(ACT.sem, ACT.cnt)
                    psA.release(pi, a1)
                    ACT.wait(a1)
                    a2 = ACT.sig(nc.scalar.activation(out=A[:, :], in_=A[:, :], func=AF.Ln, bias=1.0))
                    bi, B, bwar = tB.next()
                    DVE.wait(a2, bwar, rm_tok)
                    d1 = DVE.sig(nc.vector.tensor_tensor_scan(out=B[:, :], data0=rmask[:, :], data1=A[:, :], initial=0.0,
                                                              op0=ALU.mult, op1=ALU.add))
                    tA.release(ai, d1)
                    Blast = B[:, :].rearrange("p (c t) -> p c t", t=128)[:, :, 127]
                    ni, NB, nwar = nb.next()
                    DVE.wait(d1, nwar)
                    DVE.sig(nc.vector.tensor_scalar(out=NB[:, 0:4], in0=Blast, scalar1=-1.0 / 16, scalar2=None,
                                                    op0=ALU.mult))
                    d2 = DVE.sig(nc.vector.tensor_scalar(out=NB[:, 4:8], in0=Blast, scalar1=1.0 / 16, scalar2=None,
                                                         op0=ALU.mult))
                    ACT.wait(d1, d2)
                    ACT.sig(nc.scalar.activation(out=eb[:, h, tg * 4:(tg + 1) * 4], in_=Blast, func=AF.Exp,
                                                 scale=-1.0 / 16))
                    ci, C, cwar = tC.next()
                    di, Dt, dwar = tD.next()
                    ACT.wait(cwar, dwar)
                    for c in range(4):
                        ACT.sig(nc.scalar.activation(out=C[:, c * 128:(c + 1) * 128], in_=B[:, c * 128:(c + 1) * 128],
                                                     func=AF.Exp, scale=1.0 / 16, bias=NB[:, c:c + 1]))
                        ACT.sig(nc.scalar.activation(out=Dt[:, c * 128:(c + 1) * 128], in_=B[:, c * 128:(c + 1) * 128],
                                                     func=AF.Exp, scale=-1.0 / 16, bias=NB[:, 4 + c:5 + c]))
                    a3 = Tok(ACT.sem, ACT.cnt)
                    tB.release(bi, a3)
                    nb.release(ni, a3)
                    qi, Q, qwar = qf.next()
                    lq = self.dma(Q[:, :], self.QG[h * 128:(h + 1) * 128, t0:t0 + 512], qf.dsem[qi], waits=[qwar])
                    ki, Kt, kwar = kf.next()
                    lk = self.dma(Kt[:, :], self.KG[h * 128:(h + 1) * 128, t0:t0 + 512], kf.dsem[ki], waits=[kwar])
                    DVE.wait(a3, lq)
                    d3 = DVE.sig(nc.vector.scalar_tensor_tensor(out=qh[:, h, t0:t0 + 512], in0=Q[:, :], scalar=scale,
                                                                in1=Dt[:, :], op0=ALU.mult, op1=ALU.mult))
                    qf.release(qi, d3)
                    tD.release(di, d3)
                    POOL.wait(a3, lk)
                    p3 = POOL.sig(nc.gpsimd.tensor_tensor(out=kd[:, h, t0:t0 + 512], in0=Kt[:, :], in1=C[:, :],
                                                          op=ALU.mult))
                    kf.release(ki, p3)
                    tC.release(ci, p3)
            pre_toks = [Tok(DVE.sem, DVE.cnt), Tok(POOL.sem, POOL.cnt), Tok(ACT.sem, ACT.cnt)]
            St = self.sb([128, H, DV], F32, "St")
            Sb = Ring([self.sb([128, DV], BF16, "Sb") for _ in range(3)])
            vr = Ring([self.sb([128, GLA_V], BF16, "vch") for _ in range(2)])
            vr.dsem = [self.ds() for _ in range(2)]
            gr = Ring([self.sb([128, GLA_V], F32, "sgg") for _ in range(2)])
            gr.dsem = [self.ds() for _ in range(2)]
            oraw = self.sb([128, H, DV], F32, "oraw")
            otmp = self.sb([128, H, DV], F32, "otmp")
            ogt = self.sb([128, GLA_V], BF16, "ogt")
            ss = self.sb([128, 8], F32, "ss")
            rs = self.sb([128, 8], F32, "rs")
            junk = self.sb([128, DV], F32, "junk")
            OGT = Ring([self.sb([128, 12, 512], BF16, "OGT") for _ in range(2)])
            OGT.dsem = [self.ds() for _ in range(2)]
            Ar = Ring([self.sb([128, 128], BF16, "A") for _ in range(3)])
            Kr = Ring([self.sb([128, 128], BF16, "kdt") for _ in range(3)])
            ps_s = Ring([self.ps([128, 512], F32, "ps_s") for _ in range(1)])
            ps_sub = Ring([ps_s.tiles[0][:, i * 128:(i + 1) * 128] for i in range(4)])
            ps_tT = self.ps([128, 1024], BF16, "ps_t")
            ps_t = Ring([ps_tT[:, i * 128:(i + 1) * 128] for i in range(4)])
            ps_o = Ring([self.ps([128, 512], F32, "ps_o") for _ in range(2)])
            ps_kv = Ring([self.ps([128, 512], F32, "ps_kv") for _ in range(1)])
            ps_kv = Ring([ps_kv.tiles[0][:, 0:DV], ps_kv.tiles[0][:, 256:256 + DV]])
            ps_TT = self.ps([128, 1024], BF16, "ps_T")
            PE.wait(*pre_toks)
            DVE.wait(*pre_toks)
            ACT.wait(*pre_toks)
            st_tok = [None] * H
            ss_war = None
            otmp_war = None
            oraw_war = None
            ogt_war = None
            psT_war = None
            gi_cur = None
            for n in range(NCH):
                c0 = n * 128
                vi, V, vwar = vr.next()
                lv = self.dma(V[:, :], self.GV[c0:c0 + 128, :], vr.dsem[vi], waits=[vwar])
                gi, G, gwar = gr.next()
                lg = self.dma(G[:, :], self.GG[c0:c0 + 128, :], gr.dsem[gi], waits=[gwar])
                PE.wait(lv)
                sstoks = []
                for h in range(H):
                    qc = qh[:, h, c0:c0 + 128]
                    kc_ = kd[:, h, c0:c0 + 128]
                    si, pss, swar = ps_sub.next()
                    PE.wait(swar)
                    m_s = PE.sig(nc.tensor.matmul(pss, lhsT=kc_, rhs=qc, start=True, stop=True))
                    ti, pst, twar = ps_t.next()
                    PE.wait(twar)
                    m_t = PE.sig(nc.tensor.transpose(out=pst, in_=kc_, identity=self.ident_b))
                    ai, A, awar = Ar.next()
                    DVE.wait(m_s, awar)
                    dA = DVE.sig(nc.vector.tensor_tensor(out=A[:, :], in0=pss, in1=self.mask_f, op=ALU.mult))
                    ps_sub.release(si, dA)
                    ki, KT, kwar = Kr.next()
                    ACT.wait(m_t, kwar)
                    aK = ACT.sig(nc.scalar.activation(out=KT[:, :], in_=pst, func=AF.Copy))
                    ps_t.release(ti, aK)
                    if n > 0:
                        DVE.wait(st_tok[h])
                        dS = DVE.sig(nc.vector.tensor_scalar(out=St[:, h, :], in0=St[:, h, :], scalar1=eb[:, h, n:n + 1],
                                                             scalar2=None, op0=ALU.mult))
                        bi, SB, bwar = Sb.next()
                        ACT.wait(dS, bwar)
                        aS = ACT.sig(nc.scalar.activation(out=SB[:, :], in_=St[:, h, :], func=AF.Copy))
                    oi, pso, owar = ps_o.next()
                    PE.wait(dA, owar)
                    m_o = PE.sig(nc.tensor.matmul(pso[:, 0:DV], lhsT=A[:, :], rhs=V[:, h * DV:(h + 1) * DV],
                                                  start=True, stop=(n == 0)))
                    if n > 0:
                        PE.wait(aS)
                        m_o = PE.sig(nc.tensor.matmul(pso[:, 0:DV], lhsT=qc, rhs=SB[:, :], start=False, stop=True))
                        Sb.release(bi, m_o)
                    Ar.release(ai, m_o)
                    vi2, psk, kvwar = ps_kv.next()
                    PE.wait(aK, kvwar)
                    m_k = PE.sig(nc.tensor.matmul(psk, lhsT=KT[:, :], rhs=V[:, h * DV:(h + 1) * DV], start=True, stop=True))
                    Kr.release(ki, m_k)
                    DVE.wait(m_k)
                    if n == 0:
                        dS2 = DVE.sig(nc.vector.tensor_copy(out=St[:, h, :], in_=psk))
                    else:
                        DVE.wait(aS)
                        dS2 = DVE.sig(nc.vector.tensor_tensor(out=St[:, h, :], in0=psk, in1=St[:, h, :], op=ALU.add))
                    st_tok[h] = dS2
                    ps_kv.release(vi2, dS2)
                    ACT.wait(m_o, oraw_war, ss_war)
                    ACT.sig(nc.scalar.activation(out=oraw[:, h, :], in_=pso[:, 0:DV], func=AF.Copy))
                    a_o = ACT.sig(nc.scalar.activation(out=junk[:, :], in_=pso[:, 0:DV], func=AF.Square,
                                                       accum_out=ss[:, h:h + 1]))
                    ps_o.release(oi, a_o)
                    sstoks.append(a_o)
                vr.release(vi, Tok(PE.sem, PE.cnt))
                ACT.wait(sstoks[-1], oraw_war)
                a_r = ACT.sig(nc.scalar.activation(out=rs[:, :], in_=ss[:, :], func=AF.Sqrt, scale=1.0 / DV, bias=EPS))
                DVE.wait(a_r, lt3)
                d_r = DVE.sig(nc.vector.reciprocal(out=rs[:, :], in_=rs[:, :]))
                ss_war = a_r
                DVE.wait(d_r, otmp_war)
                for h in range(H):
                    d_n = DVE.sig(nc.vector.scalar_tensor_tensor(out=otmp[:, h, :], in0=oraw[:, h, :], scalar=rs[:, h:h + 1],
                                                                 in1=bc[:, :], op0=ALU.mult, op1=ALU.mult))
                oraw_war = d_n
                POOL.wait(d_n, lg, ogt_war)
                p_g = POOL.sig(nc.gpsimd.tensor_tensor(out=ogt[:, :], in0=otmp[:, :, :].rearrange("p h d -> p (h d)"),
                                                       in1=G[:, :], op=ALU.mult))
                gr.release(gi, p_g)
                otmp_war = p_g
                if n % 4 == 0:
                    gi_cur = OGT.next()
                oi_, OT, otwar = gi_cur
                for f0, nf in ((0, 8), (8, 4)):
                    PE.wait(p_g, psT_war)
                    for f in range(nf):
                        ins = nc.tensor.transpose(out=ps_TT[:, f * 128:(f + 1) * 128],
                                                  in_=ogt[:, (f0 + f) * 128:(f0 + f + 1) * 128], identity=self.ident_b)
                    m_T = PE.sig(ins)
                    ACT.wait(m_T, otwar if n % 4 == 0 else None)
                    a_T = ACT.sig(nc.scalar.activation(
                        out=OT[:, f0:f0 + nf, (n % 4) * 128:(n % 4 + 1) * 128],
                        in_=ps_TT[:, 0:nf * 128].rearrange("p (f t) -> p f t", t=128), func=AF.Copy))
                    psT_war = a_T
                ogt_war = Tok(PE.sem, PE.cnt)
                if n % 4 == 3:
                    q0 = (n // 4) * 512
                    stt = self.dma(self.OG.ap().rearrange("(k p) s -> p k s", p=128)[:, :, q0:q0 + 512], OT[:, :, :],
                                   OGT.dsem[oi_], waits=[a_T])
                    OGT.release(oi_, stt)

    def phase_C(self, l):
        nc, S, PE, ACT, DVE, POOL = self.nc, self.S, self.PE, self.ACT, self.DVE, self.POOL
        with self.phase():
            lng = self.sb([128, 2048], F32, "lngb")
            t1 = self.load_small(lng[:, :], self.p_bc[l, :, 448:2496])
            wsf = self.sb([128, 8, 128], F32, "wsf")
            t2 = self.load_small(wsf[:, :, :], self.p_sgw[l, :, :].rearrange("p (g t) -> p g t", t=128))
            bs = self.sb([128, 8], F32, "bs")
            t3 = self.load_small(bs[:, :], self.p_sgb[l, :, :])
            wsb = self.sb([128, 8, 128], BF16, "wsb")
            DVE.wait(t2)
            for g in range(8):
                DVE.sig(nc.vector.tensor_tensor(out=wsb[:, g, :], in0=wsf[:, g, :], in1=self.mask_f, op=ALU.mult))
            w_tok = Tok(DVE.sem, DVE.cnt)
            ur = self.make_xring(2, 1024)
            vr = self.make_xring(2, 1024)
            gu = Ring([self.sb([128, 1024], F32, "gu") for _ in range(2)])
            gv = Ring([self.sb([128, 1024], F32, "gv") for _ in range(2)])
            vn = Ring([self.sb([128, 1024], BF16, "vn") for _ in range(2)])
            junk = self.sb([128, 1024], F32, "junkC")
            stat = Ring([self.sb([128, 4], F32, "stat") for _ in range(2)])
            osg = Ring([self.sb([128, 1024], BF16, "osg") for _ in range(2)])
            psf = Ring([self.ps([128, 1024], F32, "psf") for _ in range(2)])
            psT = Ring([self.ps([128, 1024], BF16, "psTC") for _ in range(2)])
            OST = Ring([self.sb([128, 8, 512], BF16, "OST") for _ in range(2)])
            OST.dsem = [self.ds() for _ in range(2)]
            PE.wait(w_tok)
            cur = None
            for n in range(self.NCH):
                c0 = n * 128
                ui, U, uwar = ur.next()
                lu = self.dma(U[:, :], self.SU[c0:c0 + 128, :], ur.dsem[ui], waits=[uwar])
                vi, V, vwar = vr.next()
                lv = self.dma(V[:, :], self.SV[c0:c0 + 128, :], vr.dsem[vi], waits=[vwar])
                gui, GU, guwar = gu.next()
                gvi, GV_, gvwar = gv.next()
                si, ST, swar = stat.next()
                ACT.wait(lu, lv, guwar, gvwar, swar)
                a1 = ACT.sig(nc.scalar.activation(out=GU[:, :], in_=U[:, :], func=AF.Gelu_apprx_tanh))
                ur.release(ui, a1)
                a2 = ACT.sig(nc.scalar.activation(out=GV_[:, :], in_=V[:, :], func=AF.Gelu_apprx_tanh,
                                                  accum_out=ST[:, 0:1]))
                vr.release(vi, a2)
                DVE.wait(a2)
                d1 = DVE.sig(nc.vector.tensor_scalar(out=ST[:, 1:2], in0=ST[:, 0:1], scalar1=1.0 / SGW, scalar2=None,
                                                     op0=ALU.mult))
                DVE.wait(d1)
                d2 = DVE.sig(nc.vector.tensor_scalar(out=GV_[:, :], in0=GV_[:, :], scalar1=ST[:, 1:2], scalar2=None,
                                                     op0=ALU.subtract))
                ACT.wait(d2)
                a3 = ACT.sig(nc.scalar.activation(out=junk[:, :], in_=GV_[:, :], func=AF.Square, accum_out=ST[:, 2:3]))
                ACT.wait(a3)
                a4 = ACT.sig(nc.scalar.activation(out=ST[:, 3:4], in_=ST[:, 2:3], func=AF.Sqrt, scale=1.0 / SGW, bias=EPS))
                DVE.wait(a4)
                d3 = DVE.sig(nc.vector.reciprocal(out=ST[:, 3:4], in_=ST[:, 3:4]))
                DVE.wait(d3, t1)
                d4 = DVE.sig(nc.vector.scalar_tensor_tensor(out=GV_[:, :], in0=GV_[:, :], scalar=ST[:, 3:4],
                                                            in1=lng[:, 0:1024], op0=ALU.mult, op1=ALU.mult))
                stat.release(si, d4)
                ni, VN, nwar = vn.next()
                POOL.wait(d4, nwar)
                p1 = POOL.sig(nc.gpsimd.tensor_tensor(out=VN[:, :], in0=GV_[:, :], in1=lng[:, 1024:2048], op=ALU.add))
                gv.release(gvi, p1)
                fi, PF, fwar = psf.next()
                PE.wait(p1, fwar)
                for g in range(8):
                    ins = nc.tensor.matmul(PF[:, g * 128:(g + 1) * 128], lhsT=wsb[:, g, :], rhs=VN[:, g * 128:(g + 1) * 128],
                                           start=True, stop=True)
                m1 = PE.sig(ins)
                vn.release(ni, m1)
                oi, OSG, owar = osg.next()
                DVE.wait(m1, a1, owar, t3)
                for g in range(8):
                    d5 = DVE.sig(nc.vector.scalar_tensor_tensor(out=OSG[:, g * 128:(g + 1) * 128],
                                                                in0=PF[:, g * 128:(g + 1) * 128], scalar=bs[:, g:g + 1],
                                                                in1=GU[:, g * 128:(g + 1) * 128], op0=ALU.add, op1=ALU.mult))
                psf.release(fi, d5)
                gu.release(gui, d5)
                ti, PT, twar = psT.next()
                PE.wait(d5, twar)
                for g in range(8):
                    ins = nc.tensor.transpose(out=PT[:, g * 128:(g + 1) * 128], in_=OSG[:, g * 128:(g + 1) * 128],
                                              identity=self.ident_b)
                m2 = PE.sig(ins)
                osg.release(oi, m2)
                if n % 4 == 0:
                    cur = OST.next()
                oi_, OT, otwar = cur
                ACT.wait(m2, otwar if n % 4 == 0 else None)
                a5 = ACT.sig(nc.scalar.activation(out=OT[:, :, (n % 4) * 128:(n % 4 + 1) * 128],
                                                  in_=PT[:, :].rearrange("p (f t) -> p f t", t=128), func=AF.Copy))
                psT.release(ti, a5)
                if n % 4 == 3:
                    q0 = (n // 4) * 512
                    stt = self.dma(self.OS.ap().rearrange("(k p) s -> p k s", p=128)[:, :, q0:q0 + 512], OT[:, :, :],
                                   OST.dsem[oi_], waits=[a5])
                    OST.release(oi_, stt)

    def phase_D(self, l):
        nc, S, PE, ACT, DVE, POOL = self.nc, self.S, self.PE, self.ACT, self.DVE, self.POOL
        NCH = self.NCH
        H, DV = DIFF_H, DIFF_DV
        lam_init = 0.8 - 0.6 * math.exp(-0.3 * l)
        with self.phase():
            lv = self.sb([128, 512], F32, "lamv")
            t1 = self.load_small(lv[:, :], self.p_lam[l, :, :])
            lw = self.sb([128, 8], F32, "lamw")
            junk = self.sb([128, 256], F32, "junkD")
            DVE.wait(t1)
            DVE.sig(nc.vector.tensor_tensor(out=junk[:, 0:128], in0=lv[:, 0:128], in1=lv[:, 128:256], op=ALU.mult))
            DVE.sig(nc.vector.tensor_tensor(out=junk[:, 128:256], in0=lv[:, 256:384], in1=lv[:, 384:512], op=ALU.mult))
            DVE.wait(Tok(DVE.sem, DVE.cnt))
            DVE.sig(nc.vector.reduce_sum(out=lw[:, 0:1], in_=junk[:, 0:128], axis=AX.X))
            d0 = DVE.sig(nc.vector.reduce_sum(out=lw[:, 1:2], in_=junk[:, 128:256], axis=AX.X))
            ACT.wait(d0)
            a0 = ACT.sig(nc.scalar.activation(out=lw[:, 2:4], in_=lw[:, 0:2], func=AF.Exp))
            DVE.wait(a0)
            DVE.sig(nc.vector.tensor_tensor(out=lw[:, 4:5], in0=lw[:, 2:3], in1=lw[:, 3:4], op=ALU.subtract))
            DVE.wait(Tok(DVE.sem, DVE.cnt))
            DVE.sig(nc.vector.tensor_scalar(out=lw[:, 5:6], in0=lw[:, 4:5], scalar1=lam_init, scalar2=-1.0,
                                            op0=ALU.add, op1=ALU.mult))
            dn = self.sb([128, 256], F32, "dnbc")
            t2 = self.load_small(dn[:, :], self.p_bc[l, :, 192:448])
            DVE.wait(t2, Tok(DVE.sem, DVE.cnt))
            DVE.sig(nc.vector.tensor_scalar(out=dn[:, :], in0=dn[:, :], scalar1=1.0 - lam_init, scalar2=None, op0=ALU.mult))
            lam_tok = Tok(DVE.sem, DVE.cnt)
            kT = self.sb([128, 12, S], BF16, "kT")
            kd_ = self.ds()
            ksrc = self.DK.ap().rearrange("(k p) s -> p k s", p=128)
            for k0 in range(0, 12, 4):
                ktok = self.dma(kT[:, k0:k0 + 4, :], ksrc[:, k0:k0 + 4, :], kd_)
            va = self.sb([128, NCH, H, DV + 1], BF16, "vaug")
            POOL.sig(nc.gpsimd.memset(va[:, :, :, DV:DV + 1], 1.0))
            vsrc = self.DV.ap().rearrange("(n p) (h d) -> p n h d", p=128, d=DV)
            vd_ = self.ds()
            for n in range(NCH):
                vtok = self.dma(va[:, n, :, 0:DV], vsrc[:, n, :, :], vd_)
            v_tok = [vtok, Tok(POOL.sem, POOL.cnt)]
            qT = Ring([self.sb([128, 12, 512], BF16, "qT") for _ in range(2)])
            qT.dsem = [self.ds() for _ in range(2)]
            qsrc = self.DQ.ap().rearrange("(k p) s -> p k s", p=128)
            Pr = Ring([self.sb([128, 128], BF16, "P") for _ in range(6)])
            O1 = self.sb([128, 4, DV], F32, "O1")
            o1_war = [None] * 4
            odt = self.sb([128, 4, DIFF_V], BF16, "odt")
            odt_war = None
            st = Ring([self.sb([128, 4], F32, "dst") for _ in range(4)])
            ps_s = Ring([self.ps([128, 512], F32, "ps_sD") for _ in range(2)])
            ps_o = [self.ps([128, 512], F32, "ps_oD") for _ in range(4)]
            pso_war = [None] * 4
            ps_TT = self.ps([128, 1024], BF16, "ps_TD")
            psT_war = None
            ODT = Ring([self.sb([128, 12, 512], BF16, "ODT") for _ in range(2)])
            ODT.dsem = [self.ds() for _ in range(2)]
            sc = DIFF_DK ** -0.5
            PE.wait(ktok, v_tok)
            for qg in range(S // 512):
                q0 = qg * 512
                qi, Q, qwar = qT.next()
                for k0 in range(0, 12, 4):
                    qtok = self.dma(Q[:, k0:k0 + 4, :], qsrc[:, k0:k0 + 4, q0:q0 + 512], qT.dsem[qi], waits=[qwar])
                PE.wait(qtok)
                for h in range(H):
                    for m in range(2):
                        hm = h * 2 + m
                        nkb = 4 * qg + 4
                        last_mm = [None] * 4
                        for j in range(nkb):
                            i_lo = max(j, 4 * qg)
                            nq = nkb - i_lo
                            si, pss, swar = ps_s.next()
                            PE.wait(swar)
                            m_s = PE.sig(nc.tensor.matmul(pss[:, 0:nq * 128], lhsT=kT[:, hm, j * 128:(j + 1) * 128],
                                                          rhs=Q[:, hm, (i_lo - 4 * qg) * 128:512], start=True, stop=True))
                            a_p = None
                            for i in range(i_lo, nkb):
                                ii = i - 4 * qg
                                pi, Pt, pwar = Pr.next()
                                ACT.wait(m_s, pwar)
                                a_p = ACT.sig(nc.scalar.activation(
                                    out=Pt[:, :], in_=pss[:, (i - i_lo) * 128:(i - i_lo + 1) * 128], func=AF.Exp, scale=sc,
                                    bias=self.alibi[:, h * 16 + (i - j):h * 16 + (i - j) + 1]))
                                rdy = a_p
                                if i == j:
                                    POOL.wait(a_p)
                                    rdy = POOL.sig(nc.gpsimd.tensor_tensor(out=Pt[:, :], in0=Pt[:, :], in1=self.mask_b,
                                                                           op=ALU.mult))
                                PE.wait(rdy)
                                if j == 0:
                                    PE.wait(pso_war[ii])
                                mm = PE.sig(nc.tensor.matmul(ps_o[ii][:, 0:DV + 1], lhsT=Pt[:, :], rhs=va[:, j, h, :],
                                                             start=(j == 0), stop=(j == i)))
                                Pr.release(pi, mm)
                                last_mm[ii] = mm
                            ps_s.release(si, a_p)
                        for ii in range(4):
                            sti, ST, stwar = st.next()
                            DVE.wait(last_mm[ii], stwar, lam_tok)
                            d1 = DVE.sig(nc.vector.reciprocal(out=ST[:, 0:1], in_=ps_o[ii][:, DV:DV + 1]))
                            if m == 0:
                                ACT.wait(d1, o1_war[ii])
                                a1 = ACT.sig(nc.scalar.activation(out=O1[:, ii, :], in_=ps_o[ii][:, 0:DV], func=AF.Copy,
                                                                  scale=ST[:, 0:1]))
                                pso_war[ii] = a1
                                st.release(sti, a1)
                                o1_war[ii] = a1
                            else:
                                DVE.wait(d1)
                                d2 = DVE.sig(nc.vector.tensor_scalar(out=ST[:, 1:2], in0=ST[:, 0:1], scalar1=lw[:, 5:6],
                                                                     scalar2=None, op0=ALU.mult))
                                DVE.wait(d2, o1_war[ii])
                                d3 = DVE.sig(nc.vector.scalar_tensor_tensor(out=O1[:, ii, :], in0=ps_o[ii][:, 0:DV],
                                                                            scalar=ST[:, 1:2], in1=O1[:, ii, :],
                                                                            op0=ALU.mult, op1=ALU.add))
                                pso_war[ii] = d3
                                ACT.wait(d3)
                                a2 = ACT.sig(nc.scalar.activation(out=junk[:, :], in_=O1[:, ii, :], func=AF.Square,
                                                                  accum_out=ST[:, 2:3]))
                                ACT.wait(a2)
                                a3 = ACT.sig(nc.scalar.activation(out=ST[:, 3:4], in_=ST[:, 2:3], func=AF.Sqrt,
                                                                  scale=1.0 / DV, bias=EPS))
                                DVE.wait(a3)
                                d4 = DVE.sig(nc.vector.reciprocal(out=ST[:, 3:4], in_=ST[:, 3:4]))
                                DVE.wait(d4, odt_war)
                                d5 = DVE.sig(nc.vector.scalar_tensor_tensor(out=odt[:, ii, h * DV:(h + 1) * DV],
                                                                            in0=O1[:, ii, :], scalar=ST[:, 3:4], in1=dn[:, :],
                                                                            op0=ALU.mult, op1=ALU.mult))
                                st.release(sti, d5)
                                o1_war[ii] = d5
                qT.release(qi, Tok(PE.sem, PE.cnt))
                oi_, OT, otwar = ODT.next()
                PE.wait(Tok(DVE.sem, DVE.cnt))
                a_T = None
                for ii in range(4):
                    for f0, nf in ((0, 8), (8, 4)):
                        PE.wait(psT_war)
                        for f in range(nf):
                            ins = nc.tensor.transpose(out=ps_TT[:, f * 128:(f + 1) * 128],
                                                      in_=odt[:, ii, (f0 + f) * 128:(f0 + f + 1) * 128],
                                                      identity=self.ident_b)
                        m_T = PE.sig(ins)
                        ACT.wait(m_T, otwar)
                        a_T = ACT.sig(nc.scalar.activation(
                            out=OT[:, f0:f0 + nf, ii * 128:(ii + 1) * 128],
                            in_=ps_TT[:, 0:nf * 128].rearrange("p (f t) -> p f t", t=128), func=AF.Copy))
                        psT_war = a_T
                odt_war = Tok(PE.sem, PE.cnt)
                stt = self.dma(self.OD.ap().rearrange("(k p) s -> p k s", p=128)[:, :, q0:q0 + 512], OT[:, :, :],
                               ODT.dsem[oi_], waits=[a_T])
                ODT.release(oi_, stt)

    def build(self, upto=None):
        self.declare()
        self.consts()
        self.phase_init()
        if upto == "init":
            return self.nc
        for l in range(self.L):
            for nm in ("A", "B", "C", "D", "E", "F1", "G", "F2"):
                if nm == "F1":
                    self.phase_F(l, 32)
                elif nm == "F2":
                    self.phase_F(l, 96)
                else:
                    getattr(self, "phase_" + nm)(l)
                if upto == (l, nm):
                    return self.nc
        self.phase_final()
        return self.nc


def host_small_params(inp, L):
    def gl(v):
        return np.asarray(v, np.float32).reshape(32, 128).T
    out = {}
    out["gains"] = np.stack([np.concatenate([gl(inp[k][l]) for k in ("g_pre_mix", "g_post_mix", "g_pre_ffn", "g_post_ffn")],
                                            axis=1) for l in range(L)]).astype(np.float32)
    out["glr_w"] = np.stack([np.concatenate([inp["w_gla_lr"][l], inp["b_gla_lr"][l][None]], axis=0)
                             for l in range(L)]).astype(np.float32)
    out["bc"] = np.stack([np.broadcast_to(np.concatenate([inp["gla_norm_g"][l], inp["diff_norm_g"][l], inp["sg_ln_g"][l],
                                                          inp["sg_ln_b"][l]])[None], (128, 2496))
                          for l in range(L)]).astype(np.float32)
    out["sgw"] = np.stack([np.asarray(inp["sg_w_s"][l]).transpose(2, 0, 1).reshape(128, 1024) for l in range(L)]).astype(np.float32)
    out["sgb"] = np.stack([np.asarray(inp["sg_b_s"][l]).T for l in range(L)]).astype(np.float32)
    out["lam"] = np.stack([np.broadcast_to(np.concatenate([inp["diff_lambda_q1"][l], inp["diff_lambda_k1"][l],
                                                           inp["diff_lambda_q2"][l], inp["diff_lambda_k2"][l]])[None],
                                           (128, 512)) for l in range(L)]).astype(np.float32)
    out["conv"] = np.stack([np.concatenate([inp["conv_w"][l], inp["conv_b"][l][None]], axis=0)
                            .reshape(4, 96, 128).transpose(2, 1, 0).reshape(128, 384) for l in range(L)]).astype(np.float32)
    idx = np.arange(128)
    mask = (idx[:, None] <= idx[None, :]).astype(np.float32)
    ident = np.eye(128, dtype=np.float32)
    sl = np.array(alibi_slopes(DIFF_H), np.float32)
    dist = np.arange(16)
    al = sl[None, :, None] * (idx[:, None, None] - 127.0 - dist[None, None, :] * 128.0)
    out["consts"] = np.concatenate([mask, ident, al.reshape(128, 96).astype(np.float32)], axis=1).astype(np.float32)
    return {k: np.ascontiguousarray(v) for k, v in out.items()}


WNAMES = ("w_in", "w_br_gla", "w_br_sg", "w_br_diff", "w_o", "w_up", "w_down")


def host_weight_shards(inputs, NC):
    out = [dict() for _ in range(NC)]
    for n in WNAMES:
        w = np.asarray(inputs[n], np.float32)
        L, K, N = w.shape
        PW = PANEL_W[n]
        if n == "w_in":
            cut = O_LR + GLA_R
            NP = IN_COLS_P // PW
            a = w[:, :, :cut]
            b = w[:, :, cut:]
            full = np.zeros((L, K, IN_COLS_P), np.float32)
            full[:, :, :cut] = a
            full[:, :, cut + LR_PAD:] = b
            w = full
            N = IN_COLS_P
        NP = N // PW
        NPc = NP // NC
        wp = w.reshape(L, K, NP, PW)
        for c in range(NC):
            out[c][n] = np.ascontiguousarray(wp[:, :, c * NPc:(c + 1) * NPc, :].transpose(0, 2, 1, 3))
    return out
_CACHE = {}


def kernel(**inputs):
    x = np.asarray(inputs["x"], np.float32)
    B, S, _ = x.shape
    L = inputs["w_in"].shape[0]
    NC = 8
    key = (S, L, NC)
    if key not in _CACHE:
        _CACHE[key] = Builder(S, L, NC).build()
    nc = _CACHE[key]
    small = host_small_params(inputs, L)
    in_maps = []
    wsh = host_weight_shards(inputs, NC)
    for c in range(NC):
        m = {"x": np.ascontiguousarray(x[c])}
        m.update(wsh[c])
        m.update(small)
        in_maps.append(m)
    res = run_bass_kernel_spmd(nc, in_maps, core_ids=list(range(NC)))
    return np.stack([res.results[c]["y"] for c in range(NC)], axis=0).astype(np.float32)
```
